# Optimizing a Trainium2 kernel written in Bass

```python
import jax
import jax.numpy as jnp
from jax import lax
import numpy as np

D_MODEL = 2048
BATCH = 4
SEQ = 4096
DEPTH = 2

GRID_W = 64
CTX_LEN = 256
HEAD_DIM = 64
ROPE_THETA = 10000.0
EPS = 1e-6
NEG_INF = -1e30

A_HEADS = 8
A_KV_HEADS = 2
A_REP = A_HEADS // A_KV_HEADS
A_WINDOW = 128
A_BLOCK = 128

B_HEADS = 8
B_Q_RANK = 384
B_KV_RANK = 128
B_NOPE = 64
B_ROPE = 32
B_QK = B_NOPE + B_ROPE
B_V = 64
B_QBLOCK = 128

C_HEADS = 8
C_CHUNK = 128

D_HEADS = 8
NA_ROWS = 8
NA_COLS = 16

MIX_WIDTH = (A_HEADS + B_HEADS + C_HEADS + D_HEADS) * HEAD_DIM
IN_SIZES = (A_HEADS * HEAD_DIM, A_KV_HEADS * HEAD_DIM, A_KV_HEADS * HEAD_DIM,
            B_Q_RANK, B_KV_RANK, B_ROPE,
            C_HEADS * HEAD_DIM, C_HEADS * HEAD_DIM, C_HEADS * HEAD_DIM, C_HEADS * HEAD_DIM,
            D_HEADS * HEAD_DIM, D_HEADS * HEAD_DIM, D_HEADS * HEAD_DIM)
IN_COLS = sum(IN_SIZES)

D_FF = 7168
N_EXPERTS = 8
TOP_K = 2
EXPERT_FF = 7168
N_DENSE = (DEPTH + 1) // 2
N_MOE = DEPTH // 2

kernel_name = 'hybrid_parallel_heads_diffusion_block'


def rms_norm(x, gain=None):
    xf = x.astype(jnp.float32)
    y = xf * lax.rsqrt(jnp.mean(xf * xf, axis=-1, keepdims=True) + EPS)
    if gain is not None:
        y = y * gain.astype(jnp.float32)
    return y.astype(x.dtype)


def modulate(x, shift, scale):
    return x * (1.0 + scale) + shift


def axial_rope_tables(n_tokens, rot_dim):
    n_freq = rot_dim // 4
    t = jnp.arange(n_tokens, dtype=jnp.int32)
    row = (t // GRID_W).astype(jnp.float32)
    col = (t % GRID_W).astype(jnp.float32)
    freqs = ROPE_THETA ** (-jnp.arange(n_freq, dtype=jnp.float32) / n_freq)
    ang = jnp.concatenate([row[:, None] * freqs[None, :], col[:, None] * freqs[None, :]], axis=-1)
    return jnp.cos(ang), jnp.sin(ang)


def apply_rope(x, cos, sin):
    half = x.shape[-1] // 2
    x1, x2 = x[..., :half], x[..., half:]
    c = cos[None, :, None, :].astype(x.dtype)
    s = sin[None, :, None, :].astype(x.dtype)
    return jnp.concatenate([x1 * c - x2 * s, x1 * s + x2 * c], axis=-1)


def context_attention(q, k, v, scale, sink=None):
    s = jnp.einsum('bqhd,bkhd->bhqk', q, k).astype(jnp.float32) * scale
    n_keys = k.shape[1]
    if sink is not None:
        sink_logit = jnp.broadcast_to(sink.astype(jnp.float32)[None, :, None, None], s.shape[:3] + (1,))
        s = jnp.concatenate([s, sink_logit], axis=-1)
    p = jax.nn.softmax(s, axis=-1)[..., :n_keys]
    o = jnp.einsum('bhqk,bkhd->bqhd', p.astype(v.dtype), v)
    return o.reshape(o.shape[0], o.shape[1], -1)


def window_gqa_mixer(q, k, v, qc, kc, vc, q_gain, k_gain, sink, cos, sin, with_ctx_out):
    B, S, _ = q.shape
    L = kc.shape[1]
    scale = HEAD_DIM ** -0.5
    q = apply_rope(rms_norm(q.reshape(B, S, A_HEADS, HEAD_DIM), q_gain), cos, sin)
    k = apply_rope(rms_norm(k.reshape(B, S, A_KV_HEADS, HEAD_DIM), k_gain), cos, sin)
    v = v.reshape(B, S, A_KV_HEADS, HEAD_DIM)
    kc = rms_norm(kc.reshape(B, L, A_KV_HEADS, HEAD_DIM), k_gain)
    vc = vc.reshape(B, L, A_KV_HEADS, HEAD_DIM)
    sink32 = sink.astype(jnp.float32)
    nb = S // A_BLOCK
    qb = q.reshape(B, nb, A_BLOCK, A_KV_HEADS, A_REP, HEAD_DIM)

    def band(t):
        tp = jnp.pad(t, ((0, 0), (A_BLOCK, A_BLOCK), (0, 0), (0, 0)))
        tp = tp.reshape(B, nb + 2, A_BLOCK, A_KV_HEADS, HEAD_DIM)
        return jnp.concatenate([tp[:, :-2], tp[:, 1:-1], tp[:, 2:]], axis=2)

    kb, vb = band(k), band(v)
    n_band = 3 * A_BLOCK
    qi = jnp.arange(A_BLOCK)[:, None]
    kj = jnp.arange(n_band)[None, :] - A_BLOCK
    kpos = jnp.arange(nb)[:, None, None] * A_BLOCK + kj
    valid = (jnp.abs(qi - kj) <= A_WINDOW) & (kpos >= 0) & (kpos < S)
    s_win = jnp.einsum('bnqgrd,bnkgd->bgrnqk', qb, kb).astype(jnp.float32) * scale
    s_win = jnp.where(valid, s_win, NEG_INF)
    s_ctx = jnp.einsum('bnqgrd,bkgd->bgrnqk', qb, kc).astype(jnp.float32) * scale
    s_sink = jnp.broadcast_to(sink32.reshape(A_KV_HEADS, A_REP)[None, :, :, None, None, None],
                              s_win.shape[:-1] + (1,))
    p = jax.nn.softmax(jnp.concatenate([s_win, s_ctx, s_sink], axis=-1), axis=-1)
    o = (jnp.einsum('bgrnqk,bnkgd->bnqgrd', p[..., :n_band].astype(v.dtype), vb)
         + jnp.einsum('bgrnqk,bkgd->bnqgrd', p[..., n_band:n_band + L].astype(v.dtype), vc))
    o = o.reshape(B, S, A_HEADS * HEAD_DIM)
    o_ctx = None
    if with_ctx_out:
        qcr = rms_norm(qc.reshape(B, L, A_HEADS, HEAD_DIM), q_gain)
        o_ctx = context_attention(qcr, jnp.repeat(kc, A_REP, axis=2), jnp.repeat(vc, A_REP, axis=2), scale, sink32)
    return o, o_ctx


def mla_mixer(cq, ckv, kr, cq_c, ckv_c, kr_c, qa_gain, kva_gain, w_uq, w_ukv, q_gain, k_gain, cos, sin,
              with_ctx_out):
    scale = B_QK ** -0.5

    def queries(c_q, rotate):
        Bn, T, _ = c_q.shape
        q = rms_norm((rms_norm(c_q, qa_gain) @ w_uq).reshape(Bn, T, B_HEADS, B_QK), q_gain)
        if rotate:
            q = jnp.concatenate([q[..., :B_NOPE], apply_rope(q[..., B_NOPE:], cos, sin)], axis=-1)
        return q

    def keys_values(c_kv, k_rope, rotate):
        Bn, T, _ = c_kv.shape
        kv = (rms_norm(c_kv, kva_gain) @ w_ukv).reshape(Bn, T, B_HEADS, B_NOPE + B_V)
        k_shared = jnp.broadcast_to(k_rope[:, :, None, :], (Bn, T, B_HEADS, B_ROPE))
        k = rms_norm(jnp.concatenate([kv[..., :B_NOPE], k_shared], axis=-1), k_gain)
        if rotate:
            k = jnp.concatenate([k[..., :B_NOPE], apply_rope(k[..., B_NOPE:], cos, sin)], axis=-1)
        return k, kv[..., B_NOPE:]

    q = queries(cq, True)
    k, v = keys_values(ckv, kr, True)
    k_c, v_c = keys_values(ckv_c, kr_c, False)
    B, S = q.shape[0], q.shape[1]
    nq = S // B_QBLOCK
    q_blocks = jnp.moveaxis(q.reshape(B, nq, B_QBLOCK, B_HEADS, B_QK), 1, 0)

    def attend(q_blk):
        s = jnp.concatenate([jnp.einsum('bqhd,bkhd->bhqk', q_blk, k),
                             jnp.einsum('bqhd,bkhd->bhqk', q_blk, k_c)], axis=-1)
        p = jax.nn.softmax(s.astype(jnp.float32) * scale, axis=-1)
        return (jnp.einsum('bhqk,bkhd->bqhd', p[..., :S].astype(v.dtype), v)
                + jnp.einsum('bhqk,bkhd->bqhd', p[..., S:].astype(v.dtype), v_c))

    o = jnp.moveaxis(lax.map(attend, q_blocks), 0, 1).reshape(B, S, B_HEADS * B_V)
    o_ctx = context_attention(queries(cq_c, False), k_c, v_c, scale) if with_ctx_out else None
    return o, o_ctx


def retention_scan(q, k, v, log_gamma, s0, with_output):
    B, T, H, dk = q.shape
    dv = v.shape[-1]
    n = T // C_CHUNK
    kf = k.astype(jnp.float32).reshape(B, n, C_CHUNK, H, dk)
    vf = v.astype(jnp.float32).reshape(B, n, C_CHUNK, H, dv)
    pos = jnp.arange(C_CHUNK, dtype=jnp.float32)
    lg = log_gamma[:, None]
    k_w = jnp.exp(lg * (C_CHUNK - 1.0 - pos))
    kv_chunk = jnp.einsum('bnjhd,hj,bnjhe->bnhde', kf, k_w, vf)
    chunk_decay = jnp.exp(log_gamma * C_CHUNK)[None, :, None, None]

    def step(s, kv):
        return s * chunk_decay + kv, s

    s_final, s_prev = lax.scan(step, s0, jnp.moveaxis(kv_chunk, 1, 0))
    if not with_output:
        return None, s_final
    qf = q.astype(jnp.float32).reshape(B, n, C_CHUNK, H, dk)
    diff = pos[:, None] - pos[None, :]
    decay = jnp.where(diff >= 0, jnp.exp(lg[:, :, None] * jnp.maximum(diff, 0.0)), 0.0)
    scores = jnp.einsum('bnihd,bnjhd->bnhij', qf, kf) * decay
    o_intra = jnp.einsum('bnhij,bnjhe->bnihe', scores, vf)
    q_w = jnp.exp(lg * (pos + 1.0))
    o_cross = jnp.einsum('bnihd,hi,nbhde->bnihe', qf, q_w, s_prev)
    return (o_intra + o_cross).reshape(B, T, H, dv), s_final


def retention_mixer(q, k, v, g, qc, kc, vc, gc, log_decay, cos, sin, with_ctx_out):
    B, S, _ = q.shape
    L = kc.shape[1]
    k_scale = HEAD_DIM ** -0.5

    def heads(t):
        return t.reshape(t.shape[0], t.shape[1], C_HEADS, HEAD_DIM)

    q_l = apply_rope(heads(q), cos, sin)
    k_l = apply_rope(heads(k), cos, sin) * k_scale
    v_l = heads(v)
    q_c, k_c, v_c = heads(qc), heads(kc) * k_scale, heads(vc)
    log_gamma = -jnp.exp(log_decay.astype(jnp.float32))
    s0 = jnp.zeros((B, C_HEADS, HEAD_DIM, HEAD_DIM), jnp.float32)
    o = jnp.zeros((B, S, C_HEADS, HEAD_DIM), jnp.float32)
    o_c = jnp.zeros((B, L, C_HEADS, HEAD_DIM), jnp.float32) if with_ctx_out else None
    for direction in range(2):
        rev = (lambda t: t[:, ::-1]) if direction == 1 else (lambda t: t)
        oc_d, s_ctx = retention_scan(rev(q_c), rev(k_c), rev(v_c), log_gamma[direction], s0, with_ctx_out)
        o_d, _ = retention_scan(rev(q_l), rev(k_l), rev(v_l), log_gamma[direction], s_ctx, True)
        o = o + rev(o_d)
        if with_ctx_out:
            o_c = o_c + rev(oc_d)

    def finish(o_, g_):
        o_n = rms_norm(o_).reshape(o_.shape[0], o_.shape[1], -1)
        return (jax.nn.silu(g_.astype(jnp.float32)) * o_n).astype(g_.dtype)

    return finish(o, g), (finish(o_c, gc) if with_ctx_out else None)


def neighborhood_mixer(q, k, v, qc, kc, vc, q_gain, k_gain, rpb, with_ctx_out):
    B, S, _ = q.shape
    L = kc.shape[1]
    rows = S // GRID_W
    kh = min(NA_ROWS, rows)
    kw = NA_COLS
    scale = HEAD_DIM ** -0.5
    qg = rms_norm(q.reshape(B, rows, GRID_W, D_HEADS, HEAD_DIM), q_gain)
    kg = rms_norm(k.reshape(B, rows, GRID_W, D_HEADS, HEAD_DIM), k_gain)
    vg = v.reshape(B, rows, GRID_W, D_HEADS, HEAD_DIM)
    kc_ = rms_norm(kc.reshape(B, L, D_HEADS, HEAD_DIM), k_gain)
    vc_ = vc.reshape(B, L, D_HEADS, HEAD_DIM)
    r = jnp.arange(rows)
    row_idx = jnp.clip(r - kh // 2, 0, rows - kh)[:, None] + jnp.arange(kh)[None, :]
    k_band = kg[:, row_idx]
    v_band = vg[:, row_idx]
    cidx = jnp.arange(GRID_W)
    col_start = jnp.clip(cidx - kw // 2, 0, GRID_W - kw)
    col_ok = (cidx[None, :] >= col_start[:, None]) & (cidx[None, :] < col_start[:, None] + kw)
    d_row = row_idx - r[:, None] + (NA_ROWS - 1)
    d_col = jnp.clip(cidx[None, :] - cidx[:, None] + (NA_COLS - 1), 0, 2 * NA_COLS - 2)
    bias = rpb[:, d_row[:, None, :, None], d_col[None, :, None, :]]
    s_win = jnp.einsum('brwhd,brkchd->bhrwkc', qg, k_band).astype(jnp.float32) * scale
    s_win = jnp.where(col_ok[:, None, :], s_win + bias.astype(jnp.float32)[None], NEG_INF)
    n_win = kh * GRID_W
    s_win = s_win.reshape(B, D_HEADS, rows, GRID_W, n_win)
    s_ctx = jnp.einsum('brwhd,bkhd->bhrwk', qg, kc_).astype(jnp.float32) * scale
    p = jax.nn.softmax(jnp.concatenate([s_win, s_ctx], axis=-1), axis=-1)
    p_win = p[..., :n_win].reshape(B, D_HEADS, rows, GRID_W, kh, GRID_W).astype(v.dtype)
    o = (jnp.einsum('bhrwkc,brkchd->brwhd', p_win, v_band)
         + jnp.einsum('bhrwk,bkhd->brwhd', p[..., n_win:].astype(v.dtype), vc_))
    o = o.reshape(B, S, D_HEADS * HEAD_DIM)
    o_ctx = None
    if with_ctx_out:
        qcr = rms_norm(qc.reshape(B, L, D_HEADS, HEAD_DIM), q_gain)
        o_ctx = context_attention(qcr, kc_, vc_, scale)
    return o, o_ctx


def parallel_mixers(h, hc, w_in, w_out, a_q_norm, a_k_norm, a_sink, b_q_a_norm, b_kv_a_norm, b_w_uq, b_w_ukv,
                    b_q_norm, b_k_norm, c_log_decay, d_q_norm, d_k_norm, d_rpb, with_ctx_out):
    S = h.shape[1]
    split_at = [int(i) for i in np.cumsum(IN_SIZES)[:-1]]
    aq, ak, av, bq, bkv, bkr, cq, ck, cv, cg, dq, dk, dv = jnp.split(h @ w_in, split_at, axis=-1)
    aq_c, ak_c, av_c, bq_c, bkv_c, bkr_c, cq_c, ck_c, cv_c, cg_c, dq_c, dk_c, dv_c = jnp.split(
        hc @ w_in, split_at, axis=-1)
    cos_h, sin_h = axial_rope_tables(S, HEAD_DIM)
    cos_r, sin_r = axial_rope_tables(S, B_ROPE)
    oa, oa_c = window_gqa_mixer(aq, ak, av, aq_c, ak_c, av_c, a_q_norm, a_k_norm, a_sink, cos_h, sin_h,
                                with_ctx_out)
    ob, ob_c = mla_mixer(bq, bkv, bkr, bq_c, bkv_c, bkr_c, b_q_a_norm, b_kv_a_norm, b_w_uq, b_w_ukv,
                         b_q_norm, b_k_norm, cos_r, sin_r, with_ctx_out)
    oc, oc_c = retention_mixer(cq, ck, cv, cg, cq_c, ck_c, cv_c, cg_c, c_log_decay, cos_h, sin_h, with_ctx_out)
    od, od_c = neighborhood_mixer(dq, dk, dv, dq_c, dk_c, dv_c, d_q_norm, d_k_norm, d_rpb, with_ctx_out)
    y = jnp.concatenate([oa, ob, oc, od], axis=-1) @ w_out
    y_c = (jnp.concatenate([oa_c, ob_c, oc_c, od_c], axis=-1) @ w_out) if with_ctx_out else None
    return y, y_c


def swiglu(h, w_gate_up, w_down):
    gate, up = jnp.split(h @ w_gate_up, 2, axis=-1)
    return (jax.nn.silu(gate) * up) @ w_down


def moe_swiglu(h, router, w_gate_up, w_down):
    logits = (h @ router).astype(jnp.float32)
    top_val, top_idx = lax.top_k(logits, TOP_K)
    gates = jax.nn.softmax(top_val, axis=-1)
    combine = jnp.einsum('btk,btke->bte', gates, jax.nn.one_hot(top_idx, N_EXPERTS, dtype=jnp.float32))
    out = jnp.zeros_like(h)
    for e in range(N_EXPERTS):
        out = out + combine[..., e:e + 1].astype(h.dtype) * swiglu(h, w_gate_up[e], w_down[e])
    return out


def channel_mixer(t, layer, ffn_w_gate_up, ffn_w_down, moe_router, moe_w_gate_up, moe_w_down):
    i = layer // 2
    if layer % 2 == 0:
        return swiglu(t, ffn_w_gate_up[i], ffn_w_down[i])
    return moe_swiglu(t, moe_router[i], moe_w_gate_up[i], moe_w_down[i])


def setup_inputs(seed: int = 0) -> dict:
    key = jax.random.key(seed)
    keys = iter(jax.random.split(key, 40))

    def normal(shape, std):
        return jax.random.normal(next(keys), shape, jnp.float32) * std

    def gain(shape):
        return 1.0 + 0.1 * jax.random.normal(next(keys), shape, jnp.float32)

    heads_idx = jnp.arange(C_HEADS, dtype=jnp.float32)
    base_log_decay = jnp.log(-jnp.log(1.0 - 2.0 ** (-5.0 - heads_idx)))
    return {
        'x': normal((BATCH, SEQ, D_MODEL), 1.0),
        'c': normal((BATCH, D_MODEL), 1.0),
        'ctx': normal((BATCH, CTX_LEN, D_MODEL), 1.0),
        'c_ctx': normal((D_MODEL,), 1.0),
        'w_ada': normal((DEPTH, D_MODEL, 6 * D_MODEL), 0.5 * D_MODEL ** -0.5),
        'b_ada': normal((DEPTH, 6 * D_MODEL), 0.02),
        'norm_mix': gain((DEPTH, D_MODEL)),
        'norm_ffn': gain((DEPTH, D_MODEL)),
        'w_in': normal((DEPTH, D_MODEL, IN_COLS), D_MODEL ** -0.5),
        'w_out': normal((DEPTH, MIX_WIDTH, D_MODEL), MIX_WIDTH ** -0.5),
        'a_q_norm': gain((DEPTH, HEAD_DIM)),
        'a_k_norm': gain((DEPTH, HEAD_DIM)),
        'a_sink': normal((DEPTH, A_HEADS), 0.5),
        'b_q_a_norm': gain((DEPTH, B_Q_RANK)),
        'b_kv_a_norm': gain((DEPTH, B_KV_RANK)),
        'b_w_uq': normal((DEPTH, B_Q_RANK, B_HEADS * B_QK), B_Q_RANK ** -0.5),
        'b_w_ukv': normal((DEPTH, B_KV_RANK, B_HEADS * (B_NOPE + B_V)), B_KV_RANK ** -0.5),
        'b_q_norm': gain((DEPTH, B_QK)),
        'b_k_norm': gain((DEPTH, B_QK)),
        'c_log_decay': base_log_decay[None, None, :] + normal((DEPTH, 2, C_HEADS), 0.1),
        'd_q_norm': gain((DEPTH, HEAD_DIM)),
        'd_k_norm': gain((DEPTH, HEAD_DIM)),
        'd_rpb': normal((DEPTH, D_HEADS, 2 * NA_ROWS - 1, 2 * NA_COLS - 1), 0.1),
        'ffn_w_gate_up': normal((N_DENSE, D_MODEL, 2 * D_FF), D_MODEL ** -0.5),
        'ffn_w_down': normal((N_DENSE, D_FF, D_MODEL), D_FF ** -0.5),
        'moe_router': normal((N_MOE, D_MODEL, N_EXPERTS), D_MODEL ** -0.5),
        'moe_w_gate_up': normal((N_MOE, N_EXPERTS, D_MODEL, 2 * EXPERT_FF), D_MODEL ** -0.5),
        'moe_w_down': normal((N_MOE, N_EXPERTS, EXPERT_FF, D_MODEL), EXPERT_FF ** -0.5),
    }


def reference(x, c, ctx, c_ctx, w_ada, b_ada, norm_mix, norm_ffn, w_in, w_out, a_q_norm, a_k_norm, a_sink,
              b_q_a_norm, b_kv_a_norm, b_w_uq, b_w_ukv, b_q_norm, b_k_norm, c_log_decay, d_q_norm, d_k_norm,
              d_rpb, ffn_w_gate_up, ffn_w_down, moe_router, moe_w_gate_up, moe_w_down):
    xc = ctx
    silu_c = jax.nn.silu(c)
    silu_cc = jax.nn.silu(c_ctx)
    for l in range(DEPTH):
        last = l == DEPTH - 1
        mod = silu_c @ w_ada[l] + b_ada[l]
        mod_c = silu_cc @ w_ada[l] + b_ada[l]
        sh1, sc1, g1, sh2, sc2, g2 = [m[:, None, :] for m in jnp.split(mod, 6, axis=-1)]
        sh1c, sc1c, g1c, sh2c, sc2c, g2c = jnp.split(mod_c, 6, axis=-1)
        h = modulate(rms_norm(x, norm_mix[l]), sh1, sc1)
        hc = modulate(rms_norm(xc, norm_mix[l]), sh1c, sc1c)
        y, y_c = parallel_mixers(h, hc, w_in[l], w_out[l], a_q_norm[l], a_k_norm[l], a_sink[l],
                                 b_q_a_norm[l], b_kv_a_norm[l], b_w_uq[l], b_w_ukv[l], b_q_norm[l], b_k_norm[l],
                                 c_log_decay[l], d_q_norm[l], d_k_norm[l], d_rpb[l], not last)
        x = x + g1 * y
        h2 = modulate(rms_norm(x, norm_ffn[l]), sh2, sc2)
        x = x + g2 * channel_mixer(h2, l, ffn_w_gate_up, ffn_w_down, moe_router, moe_w_gate_up, moe_w_down)
        if not last:
            xc = xc + g1c * y_c
            h2c = modulate(rms_norm(xc, norm_ffn[l]), sh2c, sc2c)
            xc = xc + g2c * channel_mixer(h2c, l, ffn_w_gate_up, ffn_w_down, moe_router, moe_w_gate_up,
                                          moe_w_down)
    return x
```

```python
import numpy as np
import ml_dtypes
from contextlib import ExitStack
import concourse.bass as bass
import concourse.mybir as mybir
from concourse.bass_utils import run_bass_kernel_spmd

F32 = mybir.dt.float32
BF16 = mybir.dt.bfloat16
ALU = mybir.AluOpType
AF = mybir.ActivationFunctionType
AX = mybir.AxisListType

D = 2048
LAT = 2048
CTX = 256
T = LAT + CTX
NT = T // 128
NL = LAT // 128
NC_ = CTX // 128
S_FULL = 4096
IN_COLS = 4896
EPS = 1e-6
NEG = -30000.0
DFF = 7168
NEXP = 8

XO = {}
_off = 0
for _n, _sz in (("AKT", 2 * 64 * LAT), ("AV", LAT * 130), ("BKT", 8 * 96 * LAT), ("BV", LAT * 520),
                ("CK", LAT * 512), ("CV", LAT * 512), ("DKT", 8 * 64 * LAT), ("DV", LAT * 520)):
    XO[_n] = (_off, _sz)
    _off += _sz
NX = _off


class Buf:
    __slots__ = ("name", "wset", "readers", "multi")

    def __init__(self, name="", multi=False):
        self.name = name
        self.wset = {}
        self.readers = []
        self.multi = multi


class TT:
    __slots__ = ("t", "b")

    def __init__(self, t, b=None):
        self.t = t
        self.b = b if b is not None else Buf()


def _bufs(lst):
    out = []
    for x in lst:
        if x is None:
            continue
        if isinstance(x, TT):
            out.append(x.b)
        elif isinstance(x, Buf):
            out.append(x)
        else:
            out.extend(_bufs(x))
    return out


class Prog:
    ENG = ("pe", "act", "dve", "pool", "sp")

    def __init__(self, nc):
        self.nc = nc
        self.ops = []
        self.semkeys = {}
        self.last_eng = {}
        self.last_dma = {}
        self.barrier_deps = set()
        self.barrier_seen = {e: True for e in self.ENG}

    def _add(self, eng, fn, reads, writes, dma, semkey):
        idx = len(self.ops)
        deps = set()
        for r in reads:
            for v in r.wset.values():
                deps.add((v, False))
        for w in writes:
            if not w.multi:
                for v in w.wset.values():
                    deps.add((v, False))
            for rd in w.readers:
                deps.add((rd, True))
        real = set()
        for d, war in deps:
            o = self.ops[d]
            if (not dma) and (not o["dma"]) and o["eng"] == eng:
                if eng == "pe" or war:
                    continue
            real.add(d)
        if not self.barrier_seen[eng]:
            real |= self.barrier_deps
            self.barrier_seen[eng] = True
        self.ops.append(dict(eng=eng, fn=fn, deps=real, dma=dma, semkey=semkey))
        for r in reads:
            r.readers.append(idx)
        for w in writes:
            if w.multi:
                w.wset[semkey if dma else eng] = idx
            else:
                w.wset = {0: idx}
            w.readers = []
        if dma:
            self.last_dma[semkey] = idx
        else:
            self.last_eng[eng] = idx
        return idx

    def op(self, eng, fn, R=(), W=()):
        return self._add(eng, fn, _bufs(R), _bufs(W), False, None)

    def dma(self, eng, out, in_, R=(), W=(), store=False, **kw):
        R = _bufs(R)
        W = _bufs(W)
        key = R[0] if store else W[0]
        if key not in self.semkeys:
            self.semkeys[key] = len(self.semkeys)
        return self._add(eng, lambda e: e.dma_start(out=out, in_=in_, **kw), R, W, True, key)

    def barrier(self):
        deps = set(self.last_eng.values()) | set(self.last_dma.values())
        self.barrier_deps = deps
        self.barrier_seen = {e: False for e in self.ENG}

    def emit(self, final_bufs=()):
        nc = self.nc
        ops = self.ops
        needed = set()
        for o in ops:
            needed |= o["deps"]
        nkeys = len(self.semkeys)
        assert nkeys <= 96, nkeys
        with ExitStack() as es:
            esem = {e: es.enter_context(nc.semaphore("s_" + e)) for e in self.ENG}
            dsem = [es.enter_context(nc.semaphore("d%d" % i)) for i in range(nkeys)]
            cnt = {}
            for i, o in enumerate(ops):
                if o["dma"]:
                    s = dsem[self.semkeys[o["semkey"]]]
                    cnt[s] = cnt.get(s, 0) + 16
                    o["tok"] = (s, cnt[s], 16)
                elif i in needed:
                    s = esem[o["eng"]]
                    cnt[s] = cnt.get(s, 0) + 1
                    o["tok"] = (s, cnt[s], 1)
                else:
                    o["tok"] = None
            finals = []
            for b in _bufs(final_bufs):
                for v in b.wset.values():
                    finals.append(ops[v]["tok"])
            per = {e: [o for o in ops if o["eng"] == e] for e in self.ENG}
            self.stats = {e: len(per[e]) for e in self.ENG}
            self.maxcnt = max(cnt.values()) if cnt else 0
            block = es.enter_context(nc.Block())

            def run(ename, E):
                waited = {}
                for o in per[ename]:
                    for d in sorted(o["deps"]):
                        s, v, _ = ops[d]["tok"]
                        if waited.get(s, 0) >= v:
                            continue
                        E.wait_ge(s, v)
                        waited[s] = v
                    inst = o["fn"](E)
                    if o["tok"] is not None:
                        s, v, inc = o["tok"]
                        inst.then_inc(s, inc)
                if ename == "sp":
                    for s, v, _ in finals:
                        if waited.get(s, 0) < v:
                            E.wait_ge(s, v)
                            waited[s] = v

            @block.tensor
            def _(E):
                run("pe", E)

            @block.scalar
            def _(E):
                run("act", E)

            @block.vector
            def _(E):
                run("dve", E)

            @block.gpsimd
            def _(E):
                run("pool", E)

            @block.sync
            def _(E):
                run("sp", E)


class K:
    def __init__(self, nc, ext_in, ext_out):
        self.nc = nc
        self.P = Prog(nc)
        self.ext_in = ext_in
        self.ext_out = ext_out
        self.dram = {}
        self.sb_base = 16384
        self.sb_off = 16384
        self.nname = 0
        self.pd = [nc.alloc_psum_tensor("pd%d" % i, [128, 1024], F32) for i in range(4)]
        self.pbuf = [Buf("pb%d" % i) for i in range(8)]
        self.rr1 = 0
        self.rr2 = 0

    def d(self, name, shape=None, dt=None):
        if name in self.dram:
            return self.dram[name]
        if name in self.ext_in:
            shape, dt = self.ext_in[name]
            kind = "ExternalInput"
        elif name in self.ext_out:
            kind = "ExternalOutput"
        else:
            kind = "Internal"
        assert shape is not None, name
        t = self.nc.dram_tensor(name, list(shape), dt, kind=kind).ap()
        self.dram[name] = TT(t, Buf(name, multi=True))
        return self.dram[name]

    def sb(self, shape, dt, name=None):
        esz = 2 if dt == BF16 else 4
        n = 1
        for s in shape[1:]:
            n *= s
        nbytes = (n * esz + 31) // 32 * 32
        self.nname += 1
        t = self.nc.alloc_sbuf_tensor_at("%s_%d" % (name or "t", self.nname), list(shape), dt, offset=self.sb_off)
        self.sb_off += nbytes
        assert self.sb_off <= 229376 - 512, ("SBUF overflow", self.sb_off)
        return TT(t)

    def sb_mark(self):
        self.sb_base = self.sb_off

    def sb_reset(self):
        self.P.barrier()
        self.sb_off = self.sb_base

    def bank1(self):
        i = 4 + self.rr1 % 4
        self.rr1 += 1
        ap = self.pd[i // 2][:, (i % 2) * 512:(i % 2 + 1) * 512]
        return ap, [self.pbuf[i]]

    def bank2(self):
        i = self.rr2 % 2
        self.rr2 += 1
        return self.pd[i][:, :], [self.pbuf[2 * i], self.pbuf[2 * i + 1]]

    def half(self, i):
        return self.pd[i // 2][:, (i % 2) * 512:(i % 2 + 1) * 512], [self.pbuf[i]]

    def dbl(self, i):
        return self.pd[i][:, :], [self.pbuf[2 * i], self.pbuf[2 * i + 1]]

    def mm(self, out, lhsT, rhs, start, stop, R, W):
        if len(rhs.shape) == 3 and len(out.shape) == 2:
            out = out.rearrange("p (a b) -> p a b", b=rhs.shape[2])
        self.P.op("pe", lambda e: e.matmul(out, lhsT, rhs, start=start, stop=stop), R, W)

    def tr(self, out, in_, ident, R, W):
        self.P.op("pe", lambda e: e.transpose(out, in_, ident), R, W)

    def act(self, out, in_, func, R, W, **kw):
        self.P.op("act", lambda e: e.activation(out, in_, func, **kw), R, W)

    def tt(self, out, a, b, op, R, W, eng="dve"):
        self.P.op(eng, lambda e: e.tensor_tensor(out, a, b, op), R, W)

    def ts(self, out, a, s1, s2, op0, op1, R, W, eng="dve"):
        if op1 is None:
            self.P.op(eng, lambda e: e.tensor_scalar(out, a, s1, s2, op0), R, W)
        else:
            self.P.op(eng, lambda e: e.tensor_scalar(out, a, s1, s2, op0, op1), R, W)

    def stt(self, out, a, s, b, op0, op1, R, W, eng="dve"):
        self.P.op(eng, lambda e: e.scalar_tensor_tensor(out, a, s, b, op0, op1), R, W)

    def cp(self, out, in_, R, W, eng="dve"):
        if eng == "act":
            self.P.op("act", lambda e: e.copy(out, in_), R, W)
        else:
            self.P.op(eng, lambda e: e.tensor_copy(out, in_), R, W)

    def red(self, out, in_, R, W, op=ALU.add):
        self.P.op("dve", lambda e: e.tensor_reduce(out, in_, AX.X, op), R, W)

    def memset(self, ap, val, W, eng="pool"):
        self.P.op(eng, lambda e: e.memset(ap, val), (), W)

    def dma(self, eng, out, in_, R, W, store=False, **kw):
        self.P.dma(eng, out, in_, R, W, store=store, **kw)

    def rstd(self, out, ss, n, R, W):
        self.act(out, ss, AF.Ln, R, W, bias=self.eps.t[:ss.shape[0], 0:1], scale=1.0 / n)
        self.act(out, out, AF.Exp, W, W, scale=-0.5)


MOD_OFF = dict(sh1=0, sc1=2048, g1=4096, sh2=6144, sc2=8192, g2=10240)


def setup_consts(k):
    idf = k.sb([128, 128], F32, "idf")
    k.ident = k.sb([128, 128], BF16, "ident")
    k.eps = k.sb([128, 1], F32, "eps")
    k.dma("sp", idf.t[:], k.d("c_ident").t, [k.d("c_ident")], [idf])
    k.cp(k.ident.t[:], idf.t[:], [idf], [k.ident])
    k.memset(k.eps.t[:], EPS, [k.eps], eng="dve")
    k.sb_mark()


def phase_A(k, l):
    P = k.P
    L = str(l)
    k.sb_reset()
    ident = k.ident
    cc = k.d("cc")
    w_ada = k.d("w_ada" + L)
    b_ada = k.d("b_ada" + L)
    mod_d = k.d("mod" + L, [2, 12288], F32)
    xin = k.d("xin" + L, [T, D], F32)

    cT = k.sb([128, 16, 2], F32)
    cTb = k.sb([128, 16, 2], BF16)
    k.dma("sp", cT.t[:], cc.t.rearrange("m (p c) -> p c m", c=16), [cc], [cT], allow_slow_non_contiguous=True)
    k.act(cTb.t[:], cT.t[:], AF.Silu, [cT], [cTb])
    bt = k.sb([2, 12288], F32)
    k.dma("sp", bt.t[:], b_ada.t.partition_broadcast(2), [b_ada], [bt])
    modsb = k.sb([2, 12288], F32)
    wv = w_ada.t.rearrange("(p c) n -> p c n", c=16)
    wb = [k.sb([128, 16, 512], BF16) for _ in range(2)]
    for g in range(24):
        w = wb[g % 2]
        k.dma("pool", w.t[:], wv[:, :, g * 512:(g + 1) * 512], [w_ada], [w])
        pb, pbf = k.bank1()
        for c in range(16):
            k.mm(pb[0:2, :], cTb.t[:, c, :], w.t[:, c, :], c == 0, c == 15, [cTb, w], pbf)
        k.tt(modsb.t[:, g * 512:(g + 1) * 512], pb[0:2, :], bt.t[:, g * 512:(g + 1) * 512], ALU.add, [pbf, bt], [modsb])
    k.dma("sp", mod_d.t, modsb.t[:], [modsb], [mod_d], store=True)

    k.sb_reset()
    hT = k.sb([128, 16, T], BF16, "hT")
    hTb = [Buf() for _ in range(NT)]
    mark2 = k.sb_off
    nm = k.sb([128, D], F32)
    k.dma("sp", nm.t[:], k.d("norm_mix" + L).t.partition_broadcast(128), [k.d("norm_mix" + L)], [nm])
    GN = []
    SH = []
    for row in range(2):
        t = k.sb([128, D], F32)
        k.dma("sp", t.t[:], mod_d.t[row, MOD_OFF["sc1"]:MOD_OFF["sc1"] + D].partition_broadcast(128), [mod_d], [t])
        k.stt(t.t[:], t.t[:], 1.0, nm.t[:], ALU.add, ALU.mult, [t, nm], [t])
        GN.append(t)
        s = k.sb([128, D], F32)
        k.dma("sp", s.t[:], mod_d.t[row, MOD_OFF["sh1"]:MOD_OFF["sh1"] + D].partition_broadcast(128), [mod_d], [s])
        SH.append(s)

    xt = [k.sb([128, D], F32) for _ in range(2)]
    sq = k.sb([128, D], F32)
    tmp = k.sb([128, D], F32)
    hb = [k.sb([128, D], BF16) for _ in range(2)]
    ss = [k.sb([128, 1], F32) for _ in range(2)]
    for ti in range(NT):
        r = 0 if ti < NL else 1
        x_t = xt[ti % 2]
        s_ = ss[ti % 2]
        h_ = hb[ti % 2]
        k.dma("sp", x_t.t[:], xin.t[ti * 128:(ti + 1) * 128, :], [xin], [x_t])
        k.act(sq.t[:], x_t.t[:], AF.Square, [x_t], [sq])
        k.red(s_.t[:], sq.t[:], [sq], [s_])
        k.rstd(s_.t[:], s_.t[:], D, [s_], [s_])
        k.stt(tmp.t[:], x_t.t[:], s_.t[:, 0:1], GN[r].t[:], ALU.mult, ALU.mult, [x_t, s_, GN[r]], [tmp])
        k.tt(h_.t[:], tmp.t[:], SH[r].t[:], ALU.add, [tmp, SH[r]], [h_])
        pb, pbf = k.bank2()
        pbb = pb.bitcast(BF16).rearrange("p (c t) -> p c t", t=128)
        for c in range(16):
            k.tr(pbb[:, c, :], h_.t[:, c * 128:(c + 1) * 128], ident.t[:], [h_, ident], pbf)
        k.cp(hT.t[:, :, ti * 128:(ti + 1) * 128], pbb[:, 0:16, :], pbf, [hTb[ti]], eng="act")

    k.P.barrier()
    k.sb_off = mark2
    def bvec(name, n):
        t = k.sb([128, n], F32)
        k.dma("sp", t.t[:], k.d(name + L).t.partition_broadcast(128), [k.d(name + L)], [t])
        return t

    g_aq = bvec("a_q_norm", 64)
    g_ak = bvec("a_k_norm", 64)
    g_dq = bvec("d_q_norm", 64)
    g_dk = bvec("d_k_norm", 64)
    g_bqa = bvec("b_q_a_norm", 384)
    g_bkva = bvec("b_kv_a_norm", 128)
    g_bq = bvec("b_q_norm", 96)
    g_bk = bvec("b_k_norm", 96)
    rope64 = k.sb([128, NT, 3, 32], F32)
    k.dma("sp", rope64.t[:], k.d("rope64").t.rearrange("(n p) a b -> p n a b", p=128), [k.d("rope64")], [rope64])
    rope32 = k.sb([128, NT, 3, 16], F32)
    k.dma("sp", rope32.t[:], k.d("rope32").t.rearrange("(n p) a b -> p n a b", p=128), [k.d("rope32")], [rope32])
    w_uq = k.sb([128, 3, 768], BF16)
    k.dma("pool", w_uq.t[:], k.d("b_w_uq" + L).t.rearrange("(c p) n -> p c n", p=128), [k.d("b_w_uq" + L)], [w_uq])
    w_ukv = k.sb([128, 1024], BF16)
    k.dma("pool", w_ukv.t[:], k.d("b_w_ukv" + L).t, [k.d("b_w_ukv" + L)], [w_ukv])

    xch = k.d("xch" + L, [NX], BF16)

    def xv(name, pat, **kw):
        o, n = XO[name]
        return xch.t[o:o + n].rearrange(pat, **kw)

    X_AKT = xv("AKT", "(h d t) -> h d t", h=2, d=64)
    X_AV = xv("AV", "(t c) -> t c", c=130)
    X_BKT = xv("BKT", "(h d t) -> h d t", h=8, d=96)
    X_BV = xv("BV", "(t c) -> t c", c=520)
    X_CK = xv("CK", "(t c) -> t c", c=512)
    X_CV = xv("CV", "(t c) -> t c", c=512)
    X_DKT = xv("DKT", "(h d t) -> h d t", h=8, d=64)
    X_DV = xv("DV", "(t c) -> t c", c=520)
    C_AKT = k.d("cAKT" + L, [2, 64, CTX], BF16)
    C_AV = k.d("cAV" + L, [CTX, 130], BF16)
    C_BKT = k.d("cBKT" + L, [8, 96, CTX], BF16)
    C_BV = k.d("cBV" + L, [CTX, 520], BF16)
    C_CK = k.d("cCK" + L, [CTX, 512], BF16)
    C_CV = k.d("cCV" + L, [CTX, 512], BF16)
    C_CKT = k.d("cCKT" + L, [8, 64, CTX], BF16)
    C_DKT = k.d("cDKT" + L, [8, 64, CTX], BF16)
    C_DV = k.d("cDV" + L, [CTX, 520], BF16)
    AQT = k.d("AQT" + L, [8, 64, T], BF16)
    BQT = k.d("BQT" + L, [8, 96, T], BF16)
    CQT = k.d("CQT" + L, [8, 64, T], BF16)
    CKT = k.d("CKT" + L, [8, 64, LAT], BF16)
    CG = k.d("CG" + L, [T, 512], BF16)
    DQT = k.d("DQT" + L, [8, 64, T], BF16)

    def dbl(shape, dt):
        return [k.sb(shape, dt) for _ in range(2)]

    s_sq = dbl([128, 1024], F32)
    s_ss = dbl([128, 8], F32)
    s_qn = dbl([128, 1024], F32)
    s_qg = dbl([128, 1024], F32)
    s_ra = dbl([128, 512], F32)
    s_rb = dbl([128, 512], F32)
    s_qr = dbl([128, 8 * 96], BF16)
    s_stg = dbl([128, 8, 128], BF16)
    s_va = dbl([128, 8, 65], BF16)
    s_pl = dbl([128, 512], BF16)
    s_cq = dbl([128, 384], BF16)
    s_cqT = dbl([128, 3, 128], BF16)
    s_kr = dbl([128, 32], F32)
    s_kr2 = dbl([128, 32], F32)
    s_kr3 = dbl([128, 32], F32)
    s_ssr = dbl([128, 1], F32)
    for v in s_va:
        k.memset(v.t[:], 1.0, [v])
    cnt = [0]

    def transposes_out(src, H, dh, dstT, col0, ncol, W_dst):
        i = cnt[0]
        pb, pbf = k.bank1()
        pbb = pb.bitcast(BF16).rearrange("p (h t) -> p h t", t=128)
        for h in range(H):
            k.tr(pbb[0:dh, h, :], src[0][:, h, :], ident.t[:], [src[1], ident], pbf)
        stg = s_stg[i % 2]
        k.cp(stg.t[0:dh, 0:H, :], pbb[0:dh, 0:H, :], pbf, [stg], eng="act")
        k.dma("sp", dstT.rearrange("h d t -> d h t")[:, :, col0:col0 + ncol], stg.t[0:dh, 0:H, :], [stg], [W_dst], store=True)

    def rope_apply(dst, srcv, tab, ti, H, half, R, W, i):
        cosb = tab.t[:, ti, 0, :].unsqueeze(1).unsqueeze(1).to_broadcast([128, H, 2, half])
        sinb = tab.t[:, ti, 1, :].unsqueeze(1).to_broadcast([128, H, half])
        nsinb = tab.t[:, ti, 2, :].unsqueeze(1).to_broadcast([128, H, half])
        ra = s_ra[i % 2]
        rb = s_rb[i % 2]
        rav = ra.t[:, 0:H * 2 * half].rearrange("p (h a d) -> p h a d", h=H, a=2)
        rbv = rb.t[:, 0:H * 2 * half].rearrange("p (h a d) -> p h a d", h=H, a=2)
        k.tt(rav, srcv, cosb, ALU.mult, R + [tab], [ra])
        k.tt(rbv[:, :, 0, :], srcv[:, :, 1, :], nsinb, ALU.mult, R + [tab], [rb])
        k.tt(rbv[:, :, 1, :], srcv[:, :, 0, :], sinb, ALU.mult, R + [tab], [rb])
        k.tt(dst, rav, rbv, ALU.add, [ra, rb], W)

    def h_heads(pp, pbf, ti, H, gain, norm, rope, scale, dstT_lat, dstT_ctx, dstT_all, W_T, tok_lat=None, tok_ctx=None, W_tok=None):
        i = cnt[0]
        n = H * 64
        ppv = pp[:, 0:n].rearrange("p (h d) -> p h d", d=64)
        qg = s_qg[i % 2]
        qgv = qg.t[:, 0:n].rearrange("p (h d) -> p h d", d=64)
        qr = s_qr[i % 2]
        qrv = qr.t[:, 0:n].rearrange("p (h d) -> p h d", d=64)
        if norm:
            sq_ = s_sq[i % 2]
            ss_ = s_ss[i % 2]
            qn = s_qn[i % 2]
            k.act(sq_.t[:, 0:n], pp[:, 0:n], AF.Square, pbf, [sq_])
            k.red(ss_.t[:, 0:H], sq_.t[:, 0:n].rearrange("p (h d) -> p h d", d=64), [sq_], [ss_])
            k.rstd(ss_.t[:, 0:H], ss_.t[:, 0:H], 64, [ss_], [ss_])
            qnv = qn.t[:, 0:n].rearrange("p (h d) -> p h d", d=64)
            k.tt(qnv, ppv, ss_.t[:, 0:H].unsqueeze(2).to_broadcast([128, H, 64]), ALU.mult, pbf + [ss_], [qn])
            dst = qgv if rope else qrv
            k.tt(dst, qnv, gain.t[:, :].unsqueeze(1).to_broadcast([128, H, 64]), ALU.mult, [qn, gain], [qg if rope else qr])
        else:
            dst = qg.t[:, 0:n] if rope else qr.t[:, 0:n]
            k.act(dst, pp[:, 0:n], AF.Copy, pbf, [qg if rope else qr], scale=scale)
        if rope:
            rope_apply(qr.t[:, 0:n].rearrange("p (h a d) -> p h a d", h=H, a=2),
                       qg.t[:, 0:n].rearrange("p (h a d) -> p h a d", h=H, a=2), rope64, ti, H, 32, [qg], [qr], i)
        if dstT_all is not None:
            transposes_out((qrv, qr), H, 64, dstT_all, ti * 128, 128, W_T)
        elif ti < NL:
            transposes_out((qrv, qr), H, 64, dstT_lat, ti * 128, 128, W_T)
        else:
            transposes_out((qrv, qr), H, 64, dstT_ctx, (ti - NL) * 128, 128, W_T)
        if tok_lat is not None:
            if ti < NL:
                k.dma("sp", tok_lat[ti * 128:(ti + 1) * 128, :], qr.t[:, 0:n], [qr], [W_tok], store=True)
            else:
                k.dma("sp", tok_ctx[(ti - NL) * 128:(ti - NL + 1) * 128, :], qr.t[:, 0:n], [qr], [W_tok], store=True)

    def h_vaug(src, pbf, ti, H, dst_lat, dst_ctx, W_dst, sstride=64, soff=0):
        i = cnt[0]
        va = s_va[i % 2]
        k.cp(va.t[:, 0:H, 0:64], src, pbf, [va], eng="act")
        if ti < NL:
            k.dma("sp", dst_lat[ti * 128:(ti + 1) * 128, :], va.t[:, 0:H, :], [va], [W_dst], store=True)
        else:
            k.dma("sp", dst_ctx[(ti - NL) * 128:(ti - NL + 1) * 128, :], va.t[:, 0:H, :], [va], [W_dst], store=True)

    def h_plain(pp, pbf, ti, n, dst_lat, dst_ctx, dst_all, W_dst):
        i = cnt[0]
        pl = s_pl[i % 2]
        k.cp(pl.t[:, 0:n], pp[:, 0:n], pbf, [pl], eng="act")
        if dst_all is not None:
            k.dma("sp", dst_all[ti * 128:(ti + 1) * 128, :], pl.t[:, 0:n], [pl], [W_dst], store=True)
        elif ti < NL:
            k.dma("sp", dst_lat[ti * 128:(ti + 1) * 128, :], pl.t[:, 0:n], [pl], [W_dst], store=True)
        else:
            k.dma("sp", dst_ctx[(ti - NL) * 128:(ti - NL + 1) * 128, :], pl.t[:, 0:n], [pl], [W_dst], store=True)

    def g_aq_(pp, pbf, ti):
        h_heads(pp, pbf, ti, 8, g_aq, True, True, 1.0, None, None, AQT.t, AQT)

    def g_akv(pp, pbf, ti):
        h_heads(pp, pbf, ti, 2, g_ak, True, True, 1.0, X_AKT, C_AKT.t, None, xch if ti < NL else C_AKT)
        h_vaug(pp[:, 128:256].rearrange("p (h d) -> p h d", d=64), pbf, ti, 2,
               X_AV.rearrange("t (h c) -> t h c", c=65), C_AV.t.rearrange("t (h c) -> t h c", c=65), xch if ti < NL else C_AV)

    def g_bq_(pp, pbf, ti):
        i = cnt[0]
        sq_ = s_sq[i % 2]
        ss_ = s_ss[i % 2]
        qn = s_qn[i % 2]
        cq = s_cq[i % 2]
        cqT = s_cqT[i % 2]
        k.act(sq_.t[:, 0:384], pp[:, 0:384], AF.Square, pbf, [sq_])
        k.red(ss_.t[:, 0:1], sq_.t[:, 0:384], [sq_], [ss_])
        k.rstd(ss_.t[:, 0:1], ss_.t[:, 0:1], 384, [ss_], [ss_])
        k.stt(cq.t[:], pp[:, 0:384], ss_.t[:, 0:1], g_bqa.t[:], ALU.mult, ALU.mult, pbf + [ss_, g_bqa], [cq])
        p1, p1f = k.bank1()
        p1b = p1.bitcast(BF16).rearrange("p (c t) -> p c t", t=128)
        for c in range(3):
            k.tr(p1b[:, c, :], cq.t[:, c * 128:(c + 1) * 128], ident.t[:], [cq, ident], p1f)
        k.cp(cqT.t[:], p1b[:, 0:3, :], p1f, [cqT], eng="act")
        p2, p2f = k.bank2()
        for (c0, cn) in ((0, 512), (512, 256)):
            for c in range(3):
                k.mm(p2[:, c0:c0 + cn], cqT.t[:, c, :], w_uq.t[:, c, c0:c0 + cn], c == 0, c == 2, [cqT, w_uq], p2f)
        n = 768
        p2v = p2[:, 0:n].rearrange("p (h d) -> p h d", d=96)
        k.act(sq_.t[:, 0:n], p2[:, 0:n], AF.Square, p2f, [sq_])
        k.red(ss_.t[:, 0:8], sq_.t[:, 0:n].rearrange("p (h d) -> p h d", d=96), [sq_], [ss_])
        k.rstd(ss_.t[:, 0:8], ss_.t[:, 0:8], 96, [ss_], [ss_])
        qnv = qn.t[:, 0:n].rearrange("p (h d) -> p h d", d=96)
        k.tt(qnv, p2v, ss_.t[:, 0:8].unsqueeze(2).to_broadcast([128, 8, 96]), ALU.mult, p2f + [ss_], [qn])
        qg = s_qg[i % 2]
        qgv = qg.t[:, 0:n].rearrange("p (h d) -> p h d", d=96)
        k.tt(qgv, qnv, g_bq.t[:, :].unsqueeze(1).to_broadcast([128, 8, 96]), ALU.mult, [qn, g_bq], [qg])
        qr = s_qr[i % 2]
        qrv = qr.t[:, 0:n].rearrange("p (h d) -> p h d", d=96)
        k.cp(qrv[:, :, 0:64], qgv[:, :, 0:64], [qg], [qr])
        rope_apply(qrv[:, :, 64:96].rearrange("p h (a d) -> p h a d", a=2),
                   qgv[:, :, 64:96].rearrange("p h (a d) -> p h a d", a=2), rope32, ti, 8, 16, [qg], [qr], i)
        transposes_out((qrv, qr), 8, 96, BQT.t, ti * 128, 128, BQT)

    def g_bkv(pp, pbf, ti):
        i = cnt[0]
        sq_ = s_sq[i % 2]
        ss_ = s_ss[i % 2]
        qn = s_qn[i % 2]
        cq = s_cq[i % 2]
        cqT = s_cqT[i % 2]
        kr = s_kr[i % 2]
        kr2 = s_kr2[i % 2]
        kr3 = s_kr3[i % 2]
        ssr = s_ssr[i % 2]
        k.act(sq_.t[:, 0:160], pp[:, 0:160], AF.Square, pbf, [sq_])
        k.red(ss_.t[:, 0:1], sq_.t[:, 0:128], [sq_], [ss_])
        k.red(ssr.t[:, 0:1], sq_.t[:, 128:160], [sq_], [ssr])
        k.rstd(ss_.t[:, 0:1], ss_.t[:, 0:1], 128, [ss_], [ss_])
        k.stt(cq.t[:, 0:128], pp[:, 0:128], ss_.t[:, 0:1], g_bkva.t[:], ALU.mult, ALU.mult, pbf + [ss_, g_bkva], [cq])
        k.tt(kr.t[:], pp[:, 128:160], g_bk.t[:, 64:96], ALU.mult, pbf + [g_bk], [kr])
        p1, p1f = k.bank1()
        p1b = p1.bitcast(BF16).rearrange("p (c t) -> p c t", t=128)
        k.tr(p1b[:, 0, :], cq.t[:, 0:128], ident.t[:], [cq, ident], p1f)
        k.cp(cqT.t[:, 0, :], p1b[:, 0, :], p1f, [cqT], eng="act")
        p2, p2f = k.bank2()
        for c0 in (0, 512):
            k.mm(p2[:, c0:c0 + 512], cqT.t[:, 0, :], w_ukv.t[:, c0:c0 + 512], True, True, [cqT, w_ukv], p2f)
        p2v = p2[:, :].rearrange("p (h d) -> p h d", d=128)
        cosb = rope32.t[:, ti, 0, :].unsqueeze(1).to_broadcast([128, 2, 16])
        krv = kr.t[:, :].rearrange("p (a d) -> p a d", a=2)
        kr2v = kr2.t[:, :].rearrange("p (a d) -> p a d", a=2)
        kr3v = kr3.t[:, :].rearrange("p (a d) -> p a d", a=2)
        k.tt(kr2v, krv, cosb, ALU.mult, [kr, rope32], [kr2])
        k.tt(kr3v[:, 0, :], krv[:, 1, :], rope32.t[:, ti, 2, :], ALU.mult, [kr, rope32], [kr3])
        k.tt(kr3v[:, 1, :], krv[:, 0, :], rope32.t[:, ti, 1, :], ALU.mult, [kr, rope32], [kr3])
        k.tt(kr2.t[:], kr2.t[:], kr3.t[:], ALU.add, [kr2, kr3], [kr2])
        k.act(sq_.t[:, 0:1024], p2[:, :], AF.Square, p2f, [sq_])
        k.red(ss_.t[:, 0:8], sq_.t[:, 0:1024].rearrange("p (h d) -> p h d", d=128)[:, :, 0:64], [sq_], [ss_])
        k.ts(ss_.t[:, 0:8], ss_.t[:, 0:8], ssr.t[:, 0:1], None, ALU.add, None, [ss_, ssr], [ss_])
        k.rstd(ss_.t[:, 0:8], ss_.t[:, 0:8], 96, [ss_], [ss_])
        rb = ss_.t[:, 0:8].unsqueeze(2)
        qnv = qn.t[:, 0:512].rearrange("p (h d) -> p h d", d=64)
        k.tt(qnv, p2v[:, :, 0:64], rb.to_broadcast([128, 8, 64]), ALU.mult, p2f + [ss_], [qn])
        qr = s_qr[i % 2]
        qrv = qr.t[:, 0:768].rearrange("p (h d) -> p h d", d=96)
        k.tt(qrv[:, :, 0:64], qnv, g_bk.t[:, 0:64].unsqueeze(1).to_broadcast([128, 8, 64]), ALU.mult, [qn, g_bk], [qr])
        k.tt(qrv[:, :, 64:96], kr2.t[:, :].unsqueeze(1).to_broadcast([128, 8, 32]), rb.to_broadcast([128, 8, 32]), ALU.mult,
             [kr2, ss_], [qr])
        if ti < NL:
            transposes_out((qrv, qr), 8, 96, X_BKT, ti * 128, 128, xch)
        else:
            transposes_out((qrv, qr), 8, 96, C_BKT.t, (ti - NL) * 128, 128, C_BKT)
        h_vaug(p2v[:, :, 64:128], p2f, ti, 8, X_BV.rearrange("t (h c) -> t h c", c=65),
               C_BV.t.rearrange("t (h c) -> t h c", c=65), xch if ti < NL else C_BV)

    def g_cq_(pp, pbf, ti):
        h_heads(pp, pbf, ti, 8, None, False, True, 1.0, None, None, CQT.t, CQT)

    def g_ck_(pp, pbf, ti):
        h_heads(pp, pbf, ti, 8, None, False, True, 0.125, CKT.t, C_CKT.t, None, CKT if ti < NL else C_CKT,
                tok_lat=X_CK, tok_ctx=C_CK.t, W_tok=xch if ti < NL else C_CK)

    def g_cv_(pp, pbf, ti):
        h_plain(pp, pbf, ti, 512, X_CV, C_CV.t, None, xch if ti < NL else C_CV)

    def g_cg_(pp, pbf, ti):
        h_plain(pp, pbf, ti, 512, None, None, CG.t, CG)

    def g_dq_(pp, pbf, ti):
        h_heads(pp, pbf, ti, 8, g_dq, True, False, 1.0, None, None, DQT.t, DQT)

    def g_dk_(pp, pbf, ti):
        h_heads(pp, pbf, ti, 8, g_dk, True, False, 1.0, X_DKT, C_DKT.t, None, xch if ti < NL else C_DKT)

    def g_dv_(pp, pbf, ti):
        h_vaug(pp[:, 0:512].rearrange("p (h d) -> p h d", d=64), pbf, ti, 8, X_DV.rearrange("t (h c) -> t h c", c=65),
               C_DV.t.rearrange("t (h c) -> t h c", c=65), xch if ti < NL else C_DV)

    groups = [(0, 512, g_aq_), (512, 256, g_akv), (768, 384, g_bq_), (1152, 160, g_bkv), (1312, 512, g_cq_),
              (1824, 512, g_ck_), (2336, 512, g_cv_), (2848, 512, g_cg_), (3360, 512, g_dq_), (3872, 512, g_dk_),
              (4384, 512, g_dv_)]
    w_in = k.d("w_in" + L)
    wiv = w_in.t.rearrange("(c p) n -> p c n", p=128)
    wib = [k.sb([128, 16, 512], BF16) for _ in range(2)]
    for gi, (c0, cn, fn) in enumerate(groups):
        w = wib[gi % 2]
        k.dma("pool", w.t[:, :, 0:cn], wiv[:, :, c0:c0 + cn], [w_in], [w])
        for ti in range(NT):
            pp, pbf = k.bank1()
            for c in range(16):
                k.mm(pp[:, 0:cn], hT.t[:, c, ti * 128:(ti + 1) * 128], w.t[:, c, 0:cn], c == 0, c == 15, [hTb[ti], w], pbf)
            fn(pp, pbf, ti)
            cnt[0] += 1


def np_bf16(a):
    return a.astype(ml_dtypes.bfloat16)


def rope_tables_np(positions, rot_dim):
    n_freq = rot_dim // 4
    pos = np.asarray(positions)
    valid = pos >= 0
    t = np.where(valid, pos, 0)
    row = (t // 64).astype(np.float32)
    col = (t % 64).astype(np.float32)
    freqs = (np.float32(10000.0) ** (-np.arange(n_freq, dtype=np.float32) / np.float32(n_freq))).astype(np.float32)
    ang = np.concatenate([row[:, None] * freqs[None, :], col[:, None] * freqs[None, :]], axis=-1).astype(np.float32)
    c = np.cos(ang).astype(np.float32)
    s = np.sin(ang).astype(np.float32)
    c = np.where(valid[:, None], c, 1.0).astype(np.float32)
    s = np.where(valid[:, None], s, 0.0).astype(np.float32)
    return np.stack([c, s, -s], axis=1).astype(np.float32)


def layer_in_specs(l):
    L = str(l)
    return {
        "w_ada" + L: ([D, 6 * D], F32), "b_ada" + L: ([6 * D], F32), "norm_mix" + L: ([D], F32),
        "norm_ffn" + L: ([D], F32), "w_in" + L: ([D, IN_COLS], F32), "w_out" + L: ([D, D], F32),
        "a_q_norm" + L: ([64], F32), "a_k_norm" + L: ([64], F32), "a_sink" + L: ([8], F32),
        "b_q_a_norm" + L: ([384], F32), "b_kv_a_norm" + L: ([128], F32), "b_w_uq" + L: ([384, 768], F32),
        "b_w_ukv" + L: ([128, 1024], F32), "b_q_norm" + L: ([96], F32), "b_k_norm" + L: ([96], F32),
        "c_log_decay" + L: ([2, 8], F32), "d_q_norm" + L: ([64], F32), "d_k_norm" + L: ([64], F32),
    }


def layer_in_arrays(inputs, l):
    L = str(l)
    out = {}
    for name in ("w_ada", "b_ada", "norm_mix", "norm_ffn", "w_in", "w_out", "a_q_norm", "a_k_norm", "a_sink",
                 "b_q_a_norm", "b_kv_a_norm", "b_w_uq", "b_w_ukv", "b_q_norm", "b_k_norm", "c_log_decay",
                 "d_q_norm", "d_k_norm"):
        out[name + L] = np.ascontiguousarray(inputs[name][l])
    return out


def core_consts(hf):
    pos = np.concatenate([np.arange(hf * LAT, (hf + 1) * LAT), -np.ones(CTX, dtype=np.int64)])
    return {"rope64": rope_tables_np(pos, 64), "rope32": rope_tables_np(pos, 32),
            "c_ident": np.eye(128, dtype=np.float32)}


CONST_SPECS = {"rope64": ([T, 3, 32], F32), "rope32": ([T, 3, 16], F32), "c_ident": ([128, 128], F32)}


def build(phases, ext_in, ext_out):
    nc = bass.Bass("TRN2", target_bir_lowering=False)
    k = K(nc, ext_in, set(ext_out))
    setup_consts(k)
    for ph in phases:
        ph(k)
    k.P.emit(final_bufs=[k.dram[n] for n in ext_out])
    return nc, k


def d_needed():
    need = {}
    for tl in range(NL):
        s = set()
        for hf in range(2):
            t = hf * NL + tl
            for qr in range(2):
                r = 2 * t + qr
                st = min(max(r - 4, 0), 56)
                for kr in range(st, st + 8):
                    s.add(kr // 2 - t)
        need[tl] = sorted(s)
    return need


D_NEED = d_needed()
D_COMBOS = [(tl, dl) for tl in range(NL) for dl in D_NEED[tl]]


def d_masks_np(hf):
    out = np.full((len(D_COMBOS), 128, 128), NEG, dtype=np.float32)
    wq = np.arange(64)
    cs = np.clip(wq - 8, 0, 48)
    colok = (wq[None, :] >= cs[:, None]) & (wq[None, :] < cs[:, None] + 16)
    for ci, (tl, dl) in enumerate(D_COMBOS):
        t = hf * NL + tl
        u = t + dl
        if u < 0 or u >= 32:
            continue
        for qr in range(2):
            r = 2 * t + qr
            st = min(max(r - 4, 0), 56)
            for kr in range(2):
                ka = 2 * u + kr
                if st <= ka < st + 8:
                    blk = np.where(colok.T, 0.0, NEG)
                    out[ci, kr * 64:(kr + 1) * 64, qr * 64:(qr + 1) * 64] = blk
    return out


def d_bias_index():
    idx = np.zeros((7, 128, 128), dtype=np.int64)
    for di, dl in enumerate(range(-3, 4)):
        for kr in range(2):
            for qr in range(2):
                drow = 2 * dl + kr - qr + 7
                drow_c = min(max(drow, 0), 14)
                wk = np.arange(64)[:, None]
                wq = np.arange(64)[None, :]
                dcol = np.clip(wk - wq + 15, 0, 30)
                idx[di, kr * 64:(kr + 1) * 64, qr * 64:(qr + 1) * 64] = drow_c * 31 + dcol
    return idx


def a_masks_np(hf):
    kq = np.arange(128)
    mprev = np.where(kq[:, None] >= kq[None, :], 0.0, NEG).astype(np.float32)
    mnext = np.where(kq[:, None] <= kq[None, :], 0.0, NEG).astype(np.float32)
    allneg = np.full((128, 128), NEG, dtype=np.float32)
    return np.stack([mprev, mnext, mprev if hf == 1 else allneg, mnext if hf == 0 else allneg])


def c_consts_np(hf):
    p = np.arange(128, dtype=np.float32)
    pos = np.stack([127 - p, p, p + 1, 128 - p], axis=1).astype(np.float32)
    i = np.arange(128)
    upper = np.maximum(i[None, :] - i[:, None], 0).astype(np.float32)
    lower = np.maximum(i[:, None] - i[None, :], 0).astype(np.float32)
    flags = np.zeros((128, 2), dtype=np.float32)
    flags[:, 0] = 1.0 if hf == 1 else 0.0
    flags[:, 1] = 1.0 if hf == 0 else 0.0
    return {"c_pos": pos, "c_ul": np.stack([upper, lower]), "c_flags": flags}


def gxv(gx, r, name, pat, **kw):
    o, n = XO[name]
    return gx.t[r, o:o + n].rearrange(pat, **kw)


def phase_B1(k, l, last):
    L = str(l)
    k.sb_reset()
    ident = k.ident
    NTB = NL if last else NT
    concatT = k.sb([128, 16, T], BF16, "concatT")
    catb = [[Buf() for _ in range(4)] for _ in range(NT)]
    k.concatT = concatT
    k.catb = catb
    mark = k.sb_off
    xch = k.d("xch" + L, [NX], BF16)
    gx = k.d("gx" + L, [2, NX], BF16)

    def xv(name, pat, **kw):
        o, n = XO[name]
        return xch.t[o:o + n].rearrange(pat, **kw)

    def emit_cat(ti, m, src, hA, hB):
        pb, pbf = k.half(hA if ti % 2 == 0 else hB)
        pbb = pb.bitcast(BF16).rearrange("p (c t) -> p c t", t=128)
        for c in range(4):
            k.tr(pbb[:, c, :], src.t[:, c * 128:(c + 1) * 128], ident.t[:], [src, ident], pbf)
        k.cp(concatT.t[:, 4 * m:4 * m + 4, ti * 128:(ti + 1) * 128], pbb[:, 0:4, :], pbf, [catb[ti][m]], eng="dve")

    def normalize(O, Of, H, extra, out, den, ncols=64):
        if extra is not None:
            k.tt(den.t[:, 0:H], O[:, 0:H, 64], extra.t[:, 0:H], ALU.add, Of + [extra], [den])
        else:
            k.cp(den.t[:, 0:H], O[:, 0:H, 64], Of, [den])
        k.P.op("dve", lambda e: e.reciprocal(den.t[:, 0:H], den.t[:, 0:H]), [den], [den])
        k.tt(out.t[:, 0:H * 64].rearrange("p (h d) -> p h d", d=64), O[:, 0:H, 0:64],
             den.t[:, 0:H].unsqueeze(2).to_broadcast([128, H, 64]), ALU.mult, Of + [den], [out])

    k.P.barrier()
    k.sb_off = mark
    AQT = k.d("AQT" + L, [8, 64, T], BF16)
    AK = k.sb([64, 2, T], BF16)
    AKh = k.sb([64, 2, 2, 128], BF16)
    AVt = k.sb([128, NT, 130], BF16)
    AVh = k.sb([128, 2, 130], BF16)
    am = k.sb([128, 4, 128], BF16)
    esink = k.sb([128, 8], F32)
    k.dma("sp", AK.t[:, :, 0:LAT], xv("AKT", "(h d t) -> d h t", h=2, d=64), [xch], [AK])
    k.dma("sp", AK.t[:, :, LAT:T], k.d("cAKT" + L, [2, 64, CTX], BF16).t.rearrange("h d t -> d h t"), [k.d("cAKT" + L)], [AK])
    k.dma("sp", AKh.t[:, :, 0, :], gxv(gx, 0, "AKT", "(h d t) -> d h t", h=2, d=64)[:, :, LAT - 128:LAT], [gx], [AKh])
    k.dma("sp", AKh.t[:, :, 1, :], gxv(gx, 1, "AKT", "(h d t) -> d h t", h=2, d=64)[:, :, 0:128], [gx], [AKh])
    k.dma("sp", AVt.t[:, 0:NL, :], xv("AV", "(n p c) -> p n c", p=128, c=130), [xch], [AVt])
    k.dma("sp", AVt.t[:, NL:NT, :], k.d("cAV" + L, [CTX, 130], BF16).t.rearrange("(n p) c -> p n c", p=128), [k.d("cAV" + L)], [AVt])
    k.dma("sp", AVh.t[:, 0, :], gxv(gx, 0, "AV", "(t c) -> t c", c=130)[LAT - 128:LAT, :], [gx], [AVh])
    k.dma("sp", AVh.t[:, 1, :], gxv(gx, 1, "AV", "(t c) -> t c", c=130)[0:128, :], [gx], [AVh])
    k.dma("pool", am.t[:], k.d("c_amask").t.rearrange("m k q -> k m q"), [k.d("c_amask")], [am])
    k.dma("sp", esink.t[:], k.d("a_sink" + L).t.partition_broadcast(128), [k.d("a_sink" + L)], [esink])
    k.act(esink.t[:], esink.t[:], AF.Exp, [esink], [esink])
    qb_ = [k.sb([64, 8, 128], BF16) for _ in range(2)]
    ptb = [k.sb([128, 512], BF16) for _ in range(3)]
    outb = [k.sb([128, 512], BF16) for _ in range(2)]
    denb = [k.sb([128, 8], F32) for _ in range(2)]
    rr = 0
    for ti in range(NTB):
        q_ = qb_[ti % 2]
        k.dma("sp", q_.t[:], AQT.t.rearrange("h d t -> d h t")[:, :, ti * 128:(ti + 1) * 128], [AQT], [q_])
        O, Of = k.dbl(2 + ti % 2)
        Ov = O.rearrange("p (h c) -> p h c", c=128)
        for g in range(2):
            if ti < NL:
                ch = []
                if ti > 0:
                    ch.append((AK.t[:, g, (ti - 1) * 128:ti * 128], AK, AVt.t[:, ti - 1, g * 65:(g + 1) * 65], AVt, 0))
                else:
                    ch.append((AKh.t[:, g, 0, :], AKh, AVh.t[:, 0, g * 65:(g + 1) * 65], AVh, 2))
                ch.append((AK.t[:, g, ti * 128:(ti + 1) * 128], AK, AVt.t[:, ti, g * 65:(g + 1) * 65], AVt, None))
                if ti < NL - 1:
                    ch.append((AK.t[:, g, (ti + 1) * 128:(ti + 2) * 128], AK, AVt.t[:, ti + 1, g * 65:(g + 1) * 65], AVt, 1))
                else:
                    ch.append((AKh.t[:, g, 1, :], AKh, AVh.t[:, 1, g * 65:(g + 1) * 65], AVh, 3))
            else:
                ch = []
            for c in range(2):
                ch.append((AK.t[:, g, LAT + c * 128:LAT + (c + 1) * 128], AK, AVt.t[:, NL + c, g * 65:(g + 1) * 65], AVt, None))
            for ci, (kc, kb, vc, vb, mi) in enumerate(ch):
                S, Sf = k.half(rr % 3)
                pt = ptb[rr % 3]
                rr += 1
                k.mm(S, kc, q_.t[:, 4 * g:4 * g + 4, :], True, mi is None, [kb, q_], Sf)
                if mi is not None:
                    k.mm(S, ident.t[:], am.t[:, mi, :].unsqueeze(1).to_broadcast([128, 4, 128]), False, True, [ident, am], Sf)
                k.act(pt.t[:], S, AF.Exp, Sf, [pt], scale=0.125)
                for j in range(4):
                    k.mm(Ov[:, 4 * g + j, 0:65], pt.t[:, j * 128:(j + 1) * 128], vc, ci == 0 and j == 0, ci == len(ch) - 1, [pt, vb], Of)
        o_ = outb[ti % 2]
        normalize(Ov, Of, 8, esink, o_, denb[ti % 2])
        emit_cat(ti, 0, o_, 3, 3)

    k.P.barrier()
    k.sb_off = mark
    DQT = k.d("DQT" + L, [8, 64, T], BF16)
    DK = k.sb([64, 8, T], BF16)
    DKh = k.sb([64, 8, 6, 128], BF16)
    DVt = k.sb([128, NT, 520], BF16)
    DVh = k.sb([128, 6, 520], BF16)
    dbias = k.sb([128, 7, 8, 128], BF16)
    dmask = k.sb([128, len(D_COMBOS), 128], BF16)
    k.dma("sp", DK.t[:, :, 0:LAT], xv("DKT", "(h d t) -> d h t", h=8, d=64), [xch], [DK])
    k.dma("sp", DK.t[:, :, LAT:T], k.d("cDKT" + L, [8, 64, CTX], BF16).t.rearrange("h d t -> d h t"), [k.d("cDKT" + L)], [DK])
    for h in range(8):
        k.dma("sp", DKh.t[:, h, 0:3, :], gxv(gx, 0, "DKT", "(h d t) -> d h t", h=8, d=64)[:, h, LAT - 384:LAT].rearrange("d (n t) -> d n t", t=128), [gx], [DKh])
        k.dma("sp", DKh.t[:, h, 3:6, :], gxv(gx, 1, "DKT", "(h d t) -> d h t", h=8, d=64)[:, h, 0:384].rearrange("d (n t) -> d n t", t=128), [gx], [DKh])
    k.dma("sp", DVt.t[:, 0:NL, :], xv("DV", "(n p c) -> p n c", p=128, c=520), [xch], [DVt])
    k.dma("sp", DVt.t[:, NL:NT, :], k.d("cDV" + L, [CTX, 520], BF16).t.rearrange("(n p) c -> p n c", p=128), [k.d("cDV" + L)], [DVt])
    k.dma("sp", DVh.t[:, 0:3, :], gxv(gx, 0, "DV", "(t c) -> t c", c=520)[LAT - 384:LAT, :].rearrange("(n p) c -> p n c", p=128), [gx], [DVh])
    k.dma("sp", DVh.t[:, 3:6, :], gxv(gx, 1, "DV", "(t c) -> t c", c=520)[0:384, :].rearrange("(n p) c -> p n c", p=128), [gx], [DVh])
    k.dma("pool", dbias.t[:], k.d("dbias" + L).t.rearrange("e k h q -> k e h q"), [k.d("dbias" + L)], [dbias])
    k.ts(dbias.t[:], dbias.t[:], 8.0, None, ALU.mult, None, [dbias], [dbias])
    k.dma("pool", dmask.t[:], k.d("c_dmask").t.rearrange("m k q -> k m q"), [k.d("c_dmask")], [dmask])
    qb_ = [k.sb([64, 8, 128], BF16) for _ in range(2)]
    ptd = [k.sb([128, 1024], BF16) for _ in range(2)]
    outb = [k.sb([128, 512], BF16) for _ in range(2)]
    denb = [k.sb([128, 8], F32) for _ in range(2)]
    combo_idx = {c: i for i, c in enumerate(D_COMBOS)}
    rr = 0
    for ti in range(NTB):
        q_ = qb_[ti % 2]
        k.dma("sp", q_.t[:], DQT.t.rearrange("h d t -> d h t")[:, :, ti * 128:(ti + 1) * 128], [DQT], [q_])
        O, Of = k.dbl(2)
        Ov = O.rearrange("p (h c) -> p h c", c=128)
        ch = []
        if ti < NL:
            for dl in D_NEED[ti]:
                kt = ti + dl
                if 0 <= kt < NL:
                    ch.append((lambda h, kt=kt: DK.t[:, h, kt * 128:(kt + 1) * 128], DK, lambda h, kt=kt: DVt.t[:, kt, h * 65:(h + 1) * 65], DVt, dl))
                else:
                    hi = 3 + kt if kt < 0 else 3 + (kt - NL)
                    ch.append((lambda h, hi=hi: DKh.t[:, h, hi, :], DKh, lambda h, hi=hi: DVh.t[:, hi, h * 65:(h + 1) * 65], DVh, dl))
        for c in range(2):
            ch.append((lambda h, c=c: DK.t[:, h, LAT + c * 128:LAT + (c + 1) * 128], DK, lambda h, c=c: DVt.t[:, NL + c, h * 65:(h + 1) * 65], DVt, None))
        for ci, (kf, kb, vf, vb, dl) in enumerate(ch):
            S, Sf = k.dbl(rr % 2)
            pt = ptd[rr % 2]
            rr += 1
            for h in range(8):
                k.mm(S[:, h * 128:(h + 1) * 128], kf(h), q_.t[:, h, :], h % 4 == 0, dl is None, [kb, q_], Sf)
            if dl is not None:
                mi = combo_idx[(ti, dl)]
                for hh in range(2):
                    k.mm(S[:, hh * 512:(hh + 1) * 512], ident.t[:], dbias.t[:, dl + 3, 4 * hh:4 * hh + 4, :], False, False, [ident, dbias], Sf)
                    k.mm(S[:, hh * 512:(hh + 1) * 512], ident.t[:], dmask.t[:, mi, :].unsqueeze(1).to_broadcast([128, 4, 128]), False, True, [ident, dmask], Sf)
            k.act(pt.t[:], S, AF.Exp, Sf, [pt], scale=0.125)
            for h in range(8):
                k.mm(Ov[:, h, 0:65], pt.t[:, h * 128:(h + 1) * 128], vf(h), ci == 0 and h % 4 == 0, ci == len(ch) - 1, [pt, vb], Of)
        o_ = outb[ti % 2]
        normalize(Ov, Of, 8, None, o_, denb[ti % 2])
        emit_cat(ti, 3, o_, 6, 7)

    k.P.barrier()
    k.sb_off = mark
    BQT = k.d("BQT" + L, [8, 96, T], BF16)
    NKC = 34
    BK = k.sb([96, 8, NKC * 128], BF16)
    BVt = k.sb([128, NKC, 520], BF16)
    for r in range(2):
        for h in range(8):
            k.dma("sp", BK.t[:, h, r * LAT:(r + 1) * LAT], gxv(gx, r, "BKT", "(h d t) -> d h t", h=8, d=96)[:, h, :], [gx], [BK])
        k.dma("sp", BVt.t[:, r * NL:(r + 1) * NL, :], gxv(gx, r, "BV", "(n p c) -> p n c", p=128, c=520), [gx], [BVt])
    k.dma("sp", BK.t[:, :, 2 * LAT:2 * LAT + CTX], k.d("cBKT" + L, [8, 96, CTX], BF16).t.rearrange("h d t -> d h t"), [k.d("cBKT" + L)], [BK])
    k.dma("sp", BVt.t[:, 2 * NL:NKC, :], k.d("cBV" + L, [CTX, 520], BF16).t.rearrange("(n p) c -> p n c", p=128), [k.d("cBV" + L)], [BVt])
    bq_ = [k.sb([96, 8, 512], BF16) for _ in range(2)]
    ptb = [k.sb([128, 512], BF16) for _ in range(3)]
    outB = [k.sb([128, 4, 512], BF16) for _ in range(2)]
    denb = [k.sb([128, 4], F32) for _ in range(2)]
    scale_b = 96.0 ** -0.5
    blocks = [(qb * 512, 512, list(range(NKC))) for qb in range(4)]
    if not last:
        blocks.append((LAT, 256, [32, 33]))
    rr = 0
    oi = 0
    for bi, (q0, qn, kcs) in enumerate(blocks):
        q_ = bq_[bi % 2]
        k.dma("sp", q_.t[:, :, 0:qn], BQT.t.rearrange("h d t -> d h t")[:, :, q0:q0 + qn], [BQT], [q_])
        ob = outB[bi % 2]
        nq = qn // 128
        for h in range(8):
            O, Of = k.half(4 + oi % 2)
            dn = denb[oi % 2]
            oi += 1
            Ov = O.rearrange("p (j c) -> p j c", c=128)
            for ci, kc in enumerate(kcs):
                S, Sf = k.half(rr % 3)
                pt = ptb[rr % 3]
                rr += 1
                k.mm(S[:, 0:qn], BK.t[:, h, kc * 128:(kc + 1) * 128], q_.t[:, h, 0:qn], True, True, [BK, q_], Sf)
                k.act(pt.t[:, 0:qn], S[:, 0:qn], AF.Exp, Sf, [pt], scale=scale_b)
                for j in range(nq):
                    k.mm(Ov[:, j, 0:65], pt.t[:, j * 128:(j + 1) * 128], BVt.t[:, kc, h * 65:(h + 1) * 65], ci == 0 and j == 0, ci == len(kcs) - 1, [pt, BVt], Of)
            k.cp(dn.t[:, 0:nq], Ov[:, 0:nq, 64], Of, [dn])
            k.P.op("dve", lambda e, dn=dn, nq=nq: e.reciprocal(dn.t[:, 0:nq], dn.t[:, 0:nq]), [dn], [dn])
            k.tt(ob.t[:, 0:nq, h * 64:(h + 1) * 64], Ov[:, 0:nq, 0:64], dn.t[:, 0:nq].unsqueeze(2).to_broadcast([128, nq, 64]), ALU.mult, Of + [dn], [ob])
        for j in range(nq):
            ti = q0 // 128 + j
            src = TT(ob.t[:, j, :], ob.b)
            pb, pbf = k.half(6 + ti % 2)
            pbb = pb.bitcast(BF16).rearrange("p (c t) -> p c t", t=128)
            for c in range(4):
                k.tr(pbb[:, c, :], ob.t[:, j, c * 128:(c + 1) * 128], ident.t[:], [ob, ident], pbf)
            k.cp(concatT.t[:, 4:8, ti * 128:(ti + 1) * 128], pbb[:, 0:4, :], pbf, [catb[ti][1]], eng="dve")

    k.P.barrier()
    k.sb_off = mark
    CQT = k.d("CQT" + L, [8, 64, T], BF16)
    CKT = k.d("CKT" + L, [8, 64, LAT], BF16)
    cCKT = k.d("cCKT" + L, [8, 64, CTX], BF16)
    CG = k.d("CG" + L, [T, 512], BF16)
    cCK = k.d("cCK" + L, [CTX, 512], BF16)
    cCV = k.d("cCV" + L, [CTX, 512], BF16)
    Kt = k.sb([128, NT, 512], BF16)
    Vt = k.sb([128, NT, 512], BF16)
    k.dma("sp", Kt.t[:, 0:NL, :], xv("CK", "(n p c) -> p n c", p=128, c=512), [xch], [Kt])
    k.dma("sp", Kt.t[:, NL:NT, :], cCK.t.rearrange("(n p) c -> p n c", p=128), [cCK], [Kt])
    k.dma("sp", Vt.t[:, 0:NL, :], xv("CV", "(n p c) -> p n c", p=128, c=512), [xch], [Vt])
    k.dma("sp", Vt.t[:, NL:NT, :], cCV.t.rearrange("(n p) c -> p n c", p=128), [cCV], [Vt])
    lg = k.sb([128, 16], F32)
    k.dma("sp", lg.t[:], k.d("c_log_decay" + L).t.rearrange("a h -> (a h)").partition_broadcast(128), [k.d("c_log_decay" + L)], [lg])
    k.act(lg.t[:], lg.t[:], AF.Exp, [lg], [lg])
    k.ts(lg.t[:], lg.t[:], -1.0, None, ALU.mult, None, [lg], [lg])
    cpos = k.sb([128, 4], F32)
    k.dma("sp", cpos.t[:], k.d("c_pos").t, [k.d("c_pos")], [cpos])
    flags = k.sb([128, 2], F32)
    k.dma("sp", flags.t[:], k.d("c_flags").t, [k.d("c_flags")], [flags])
    cul = k.sb([128, 2, 128], F32)
    k.dma("sp", cul.t[:], k.d("c_ul").t.rearrange("a j i -> j a i"), [k.d("c_ul")], [cul])
    kw = k.sb([128, 16], F32)
    qw = k.sb([128, 16], F32)
    cd = k.sb([128, 16], F32)
    for dr in range(2):
        k.ts(kw.t[:, dr * 8:(dr + 1) * 8], lg.t[:, dr * 8:(dr + 1) * 8], cpos.t[:, dr:dr + 1], None, ALU.mult, None, [lg, cpos], [kw])
        k.ts(qw.t[:, dr * 8:(dr + 1) * 8], lg.t[:, dr * 8:(dr + 1) * 8], cpos.t[:, 2 + dr:3 + dr], None, ALU.mult, None, [lg, cpos], [qw])
    k.act(kw.t[:], kw.t[:], AF.Exp, [kw], [kw])
    k.act(qw.t[:], qw.t[:], AF.Exp, [qw], [qw])
    k.act(cd.t[:], lg.t[:], AF.Exp, [lg], [cd], scale=128.0)
    DT = k.sb([128, 8, 128], F32)
    dtmp = k.sb([128, 128], F32)
    idf32 = k.sb([128, 128], F32)
    k.cp(idf32.t[:], ident.t[:], [ident], [idf32])
    for h in range(8):
        k.ts(dtmp.t[:], cul.t[:, 0, :], lg.t[:, h:h + 1], None, ALU.mult, None, [cul, lg], [dtmp])
        k.stt(dtmp.t[:], cul.t[:, 1, :], lg.t[:, 8 + h:9 + h], dtmp.t[:], ALU.mult, ALU.add, [cul, lg, dtmp], [dtmp])
        k.act(dtmp.t[:], dtmp.t[:], AF.Exp, [dtmp], [dtmp])
        k.tt(DT.t[:, h, :], dtmp.t[:], idf32.t[:], ALU.add, [dtmp, idf32], [DT])
    St = k.sb([64, 16, 64], F32)
    St0 = k.sb([64, 16, 64], F32)
    Sbf = k.sb([64, NT, 16, 64], BF16)
    zeros_b = None
    k.memset(St.t[:], 0.0, [St], eng="dve")
    k.memset(Sbf.t[:, NL:NT, :, :], 0.0, [Sbf], eng="dve")
    kwb = [k.sb([128, 512], BF16) for _ in range(2)]
    pk = [k.sb([128, 512], BF16) for _ in range(2)]
    pv = [k.sb([128, 512], BF16) for _ in range(2)]
    cnt = [0]

    def kv_update(Kap, Kb, Vap, Vb, dr, into):
        i = cnt[0]
        cnt[0] += 1
        kwt = kwb[i % 2]
        k.tt(kwt.t[:].rearrange("p (h d) -> p h d", d=64), Kap.rearrange("p (h d) -> p h d", d=64),
             kw.t[:, dr * 8:(dr + 1) * 8].unsqueeze(2).to_broadcast([128, 8, 64]), ALU.mult, [Kb, kw], [kwt])
        pb, pbf = k.half(i % 2)
        pbv = pb.rearrange("p (h e) -> p h e", e=64)
        for h in range(8):
            k.mm(pbv[0:64, h, :], kwt.t[:, h * 64:(h + 1) * 64], Vap[:, h * 64:(h + 1) * 64], True, True, [kwt, Vb], pbf)
        sl = into.t[:, dr * 8:(dr + 1) * 8, :]
        k.tt(sl, sl, cd.t[0:64, dr * 8:(dr + 1) * 8].unsqueeze(2).to_broadcast([64, 8, 64]), ALU.mult, [into, cd], [into])
        k.tt(sl, sl, pbv[0:64, :, :], ALU.add, [into] + pbf, [into])

    def snap(ti, dr):
        k.cp(Sbf.t[:, ti, dr * 8:(dr + 1) * 8, :], St.t[:, dr * 8:(dr + 1) * 8, :], [St], [Sbf])

    kv_update(Kt.t[:, NL, :], Kt, Vt.t[:, NL, :], Vt, 0, St)
    snap(NL + 1, 0)
    kv_update(Kt.t[:, NL + 1, :], Kt, Vt.t[:, NL + 1, :], Vt, 0, St)
    kv_update(Kt.t[:, NL + 1, :], Kt, Vt.t[:, NL + 1, :], Vt, 1, St)
    snap(NL, 1)
    kv_update(Kt.t[:, NL, :], Kt, Vt.t[:, NL, :], Vt, 1, St)
    k.cp(St0.t[:], St.t[:], [St], [St0])
    for dr, r, order in ((0, 0, range(NL)), (1, 1, range(NL - 1, -1, -1))):
        for m in order:
            a = pk[cnt[0] % 2]
            b = pv[cnt[0] % 2]
            k.dma("sp", a.t[:], gxv(gx, r, "CK", "(t c) -> t c", c=512)[m * 128:(m + 1) * 128, :], [gx], [a])
            k.dma("sp", b.t[:], gxv(gx, r, "CV", "(t c) -> t c", c=512)[m * 128:(m + 1) * 128, :], [gx], [b])
            kv_update(a.t[:], a, b.t[:], b, dr, St)
    for dr in range(2):
        sl = St.t[:, dr * 8:(dr + 1) * 8, :]
        s0 = St0.t[:, dr * 8:(dr + 1) * 8, :]
        k.tt(sl, sl, s0, ALU.subtract, [St, St0], [St])
        k.stt(sl, sl, flags.t[0:64, dr:dr + 1], s0, ALU.mult, ALU.add, [St, flags, St0], [St])
    for n in range(NL):
        snap(n, 0)
        if n < NL - 1:
            kv_update(Kt.t[:, n, :], Kt, Vt.t[:, n, :], Vt, 0, St)
    for n in range(NL - 1, -1, -1):
        snap(n, 1)
        if n > 0:
            kv_update(Kt.t[:, n, :], Kt, Vt.t[:, n, :], Vt, 1, St)
    qtb = [k.sb([64, 8, 128], BF16) for _ in range(2)]
    ktb = [k.sb([64, 8, 128], BF16) for _ in range(2)]
    gtb = [k.sb([128, 512], BF16) for _ in range(2)]
    s2b = [k.sb([128, 8, 128], BF16) for _ in range(2)]
    t1 = k.sb([128, 16, 64], F32)
    acc = k.sb([128, 8, 64], F32)
    sqc = k.sb([128, 512], F32)
    ssc = [k.sb([128, 8], F32) for _ in range(2)]
    sg = [k.sb([128, 512], F32) for _ in range(2)]
    outc = [k.sb([128, 512], BF16) for _ in range(2)]
    for ti in range(NTB):
        q_ = qtb[ti % 2]
        kt_ = ktb[ti % 2]
        g_ = gtb[ti % 2]
        k.dma("sp", q_.t[:], CQT.t.rearrange("h d t -> d h t")[:, :, ti * 128:(ti + 1) * 128], [CQT], [q_])
        if ti < NL:
            k.dma("sp", kt_.t[:], CKT.t.rearrange("h d t -> d h t")[:, :, ti * 128:(ti + 1) * 128], [CKT], [kt_])
        else:
            k.dma("sp", kt_.t[:], cCKT.t.rearrange("h d t -> d h t")[:, :, (ti - NL) * 128:(ti - NL + 1) * 128], [cCKT], [kt_])
        k.dma("sp", g_.t[:], CG.t[ti * 128:(ti + 1) * 128, :], [CG], [g_])
        S, Sf = k.dbl(0)
        for h in range(8):
            k.mm(S[:, h * 128:(h + 1) * 128], kt_.t[:, h, :], q_.t[:, h, :], True, True, [kt_, q_], Sf)
        s2 = s2b[ti % 2]
        k.tt(s2.t[:], S.rearrange("p (h i) -> p h i", i=128), DT.t[:], ALU.mult, Sf + [DT], [s2])
        O, Of = k.dbl(2)
        Ov = O.rearrange("p (h c) -> p h c", c=128)
        for h in range(8):
            k.mm(Ov[:, h, 0:64], s2.t[:, h, :], Vt.t[:, ti, h * 64:(h + 1) * 64], True, True, [s2, Vt], Of)
        X, Xf = k.dbl(1)
        Xv = X.rearrange("p (a e) -> p a e", e=64)
        for a in range(16):
            k.mm(Xv[:, a, :], q_.t[:, a % 8, :], Sbf.t[:, ti, a, :], True, True, [q_, Sbf], Xf)
        k.tt(t1.t[:], Xv, qw.t[:, :].unsqueeze(2).to_broadcast([128, 16, 64]), ALU.mult, Xf + [qw], [t1])
        k.tt(acc.t[:], t1.t[:, 0:8, :], t1.t[:, 8:16, :], ALU.add, [t1], [acc])
        k.tt(acc.t[:], acc.t[:], Ov[:, :, 0:64], ALU.add, [acc] + Of, [acc])
        accf = acc.t[:].rearrange("p h d -> p (h d)")
        k.act(sqc.t[:], accf, AF.Square, [acc], [sqc])
        ss_ = ssc[ti % 2]
        k.red(ss_.t[:], sqc.t[:].rearrange("p (h d) -> p h d", d=64), [sqc], [ss_])
        k.rstd(ss_.t[:], ss_.t[:], 64, [ss_], [ss_])
        sg_ = sg[ti % 2]
        k.act(sg_.t[:], g_.t[:], AF.Silu, [g_], [sg_])
        k.tt(acc.t[:], acc.t[:], ss_.t[:, :].unsqueeze(2).to_broadcast([128, 8, 64]), ALU.mult, [acc, ss_], [acc])
        o_ = outc[ti % 2]
        k.tt(o_.t[:], accf, sg_.t[:], ALU.mult, [acc, sg_], [o_])
        emit_cat(ti, 2, o_, 6, 7)


def phase_B2(k, l, last):
    L = str(l)
    NTB = NL if last else NT
    concatT, catb = k.concatT, k.catb
    k.P.barrier()
    k.sb_off = k.sb_base + 16 * T * 2
    mod_d = k.d("mod" + L, [2, 12288], F32)
    xin = k.d("xin" + L, [T, D], F32)
    x1 = k.d("x1_" + L, [T, D], F32)
    w_out = k.d("w_out" + L)
    wv = w_out.t.rearrange("(c p) n -> p c n", p=128)
    G1 = []
    for row in range(1 if last else 2):
        t = k.sb([128, D], F32)
        k.dma("sp", t.t[:], mod_d.t[row, MOD_OFF["g1"]:MOD_OFF["g1"] + D].partition_broadcast(128), [mod_d], [t])
        G1.append(t)
    wb = [k.sb([128, 16, 1024], BF16) for _ in range(2)]
    xt = [k.sb([128, 1024], F32) for _ in range(2)]
    xo = [k.sb([128, 1024], F32) for _ in range(2)]
    tmp = [k.sb([128, 512], F32) for _ in range(2)]
    i = 0
    for hh in range(2):
        w = wb[hh]
        k.dma("pool", w.t[:], wv[:, :, hh * 1024:(hh + 1) * 1024], [w_out], [w])
        for ti in range(NTB):
            r = 0 if ti < NL else 1
            x_ = xt[ti % 2]
            o_ = xo[ti % 2]
            k.dma("sp", x_.t[:], xin.t[ti * 128:(ti + 1) * 128, hh * 1024:(hh + 1) * 1024], [xin], [x_])
            for cg in range(2):
                pp, pf = k.bank1()
                for c in range(16):
                    k.mm(pp, concatT.t[:, c, ti * 128:(ti + 1) * 128], w.t[:, c, cg * 512:(cg + 1) * 512], c == 0, c == 15,
                         [catb[ti][c // 4], w], pf)
                t_ = tmp[i % 2]
                i += 1
                col = hh * 1024 + cg * 512
                k.tt(t_.t[:], pp, G1[r].t[:, col:col + 512], ALU.mult, pf + [G1[r]], [t_])
                k.tt(o_.t[:, cg * 512:(cg + 1) * 512], t_.t[:], x_.t[:, cg * 512:(cg + 1) * 512], ALU.add, [t_, x_], [o_])
            k.dma("sp", x1.t[ti * 128:(ti + 1) * 128, hh * 1024:(hh + 1) * 1024], o_.t[:], [o_], [x1], store=True)


def phase_B3(k, l, last):
    L = str(l)
    moe = (l % 2 == 1)
    k.sb_reset()
    ident = k.ident
    mod_d = k.d("mod" + L, [2, 12288], F32)
    x1 = k.d("x1_" + L, [T, D], F32)
    xout = k.d("xout" + L, [LAT if last else T, D], F32)
    groups = [(0, 8, 0), (8, 8, 0)] + ([] if last else [(16, 2, 1)])
    if moe:
        wgu_d = k.d("moe_w_gate_up")
        wdn_d = k.d("moe_w_down")
        router = k.sb([128, 16, 8], F32)
        k.dma("sp", router.t[:], k.d("moe_router").t.rearrange("(c p) e -> p c e", p=128), [k.d("moe_router")], [router])
        idf = k.sb([128, 128], F32)
        k.dma("sp", idf.t[:], k.d("c_ident").t, [k.d("c_ident")], [idf])
        h2Tf = k.sb([128, 8, 128], F32)
        lgt = k.sb([128, 8, 8], F32)
        comb = k.sb([128, 8, 8], F32)
        m1 = k.sb([128, 8], F32)
        m2 = k.sb([128, 8], F32)
        eq = k.sb([128, 8], F32)
    else:
        wgu_d = k.d("ffn_w_gate_up")
        wdn_d = k.d("ffn_w_down")
    nexp = NEXP if moe else 1
    nm = k.sb([128, D], F32)
    GN = k.sb([128, D], F32)
    SH = k.sb([128, D], F32)
    G2 = k.sb([128, D], F32)
    xacc = [k.sb([128, D], F32) for _ in range(8)]
    h2T = k.sb([128, 16, 1024], BF16)
    h2Tb = [Buf() for _ in range(8)]
    actT = k.sb([128, 7, 1024], BF16)
    wgb = [k.sb([128, 16, 256], BF16) for _ in range(2)]
    wdb = [k.sb([128, 7, 512], BF16) for _ in range(2)]
    sgb = [k.sb([128, 512], BF16) for _ in range(2)]
    tmpb = [k.sb([128, 512], F32) for _ in range(2)]
    t8a = k.sb([128, D], F32)
    t8b = k.sb([128, D], F32)
    hb = k.sb([128, D], BF16)
    ssb = [k.sb([128, 1], F32) for _ in range(2)]
    k.dma("sp", nm.t[:], k.d("norm_ffn" + L).t.partition_broadcast(128), [k.d("norm_ffn" + L)], [nm])
    rrp = [0]

    def pbank():
        i = rrp[0] % 4
        rrp[0] += 1
        return k.half(i)

    wi = [0, 0]
    for (t0, nt, row) in groups:
        ntok = nt * 128
        k.dma("sp", GN.t[:], mod_d.t[row, MOD_OFF["sc2"]:MOD_OFF["sc2"] + D].partition_broadcast(128), [mod_d], [GN])
        k.stt(GN.t[:], GN.t[:], 1.0, nm.t[:], ALU.add, ALU.mult, [GN, nm], [GN])
        k.dma("sp", SH.t[:], mod_d.t[row, MOD_OFF["sh2"]:MOD_OFF["sh2"] + D].partition_broadcast(128), [mod_d], [SH])
        k.dma("sp", G2.t[:], mod_d.t[row, MOD_OFF["g2"]:MOD_OFF["g2"] + D].partition_broadcast(128), [mod_d], [G2])
        for j in range(nt):
            ti = t0 + j
            xa = xacc[j]
            s_ = ssb[j % 2]
            k.dma("sp", xa.t[:], x1.t[ti * 128:(ti + 1) * 128, :], [x1], [xa])
            k.act(t8a.t[:], xa.t[:], AF.Square, [xa], [t8a])
            k.red(s_.t[:], t8a.t[:], [t8a], [s_])
            k.rstd(s_.t[:], s_.t[:], D, [s_], [s_])
            k.stt(t8a.t[:], xa.t[:], s_.t[:, 0:1], GN.t[:], ALU.mult, ALU.mult, [xa, s_, GN], [t8a])
            k.tt(t8b.t[:], t8a.t[:], SH.t[:], ALU.add, [t8a, SH], [t8b])
            k.cp(hb.t[:], t8b.t[:], [t8b], [hb], eng="dve")
            pb, pbf = k.bank2()
            pbb = pb.bitcast(BF16).rearrange("p (c t) -> p c t", t=128)
            for c in range(16):
                k.tr(pbb[:, c, :], hb.t[:, c * 128:(c + 1) * 128], ident.t[:], [hb, ident], pbf)
            k.cp(h2T.t[:, :, j * 128:(j + 1) * 128], pbb[:, 0:16, :], pbf, [h2Tb[j]], eng="act")
            if moe:
                pl, plf = k.bank1()
                for hh in range(2):
                    pr, prf = k.bank2()
                    prv = pr.rearrange("p (c t) -> p c t", t=128)
                    for c in range(8):
                        k.tr(prv[:, c, :], t8b.t[:, (hh * 8 + c) * 128:(hh * 8 + c + 1) * 128], idf.t[:], [t8b, idf], prf)
                    k.cp(h2Tf.t[:], prv, prf, [h2Tf], eng="act")
                    for c in range(8):
                        k.mm(pl[:, 0:8], h2Tf.t[:, c, :], router.t[:, hh * 8 + c, :], hh == 0 and c == 0, hh == 1 and c == 7, [h2Tf, router], plf)
                k.cp(lgt.t[:, j, :], pl[:, 0:8], plf, [lgt])
                lj = lgt.t[:, j, :]
                k.red(m1.t[:, j:j + 1], lj, [lgt], [m1], op=ALU.max)
                k.ts(eq.t[:], lj, m1.t[:, j:j + 1], None, ALU.is_equal, None, [lgt, m1], [eq])
                k.stt(eq.t[:], eq.t[:], -1e30, lj, ALU.mult, ALU.add, [eq, lgt], [eq])
                k.red(m2.t[:, j:j + 1], eq.t[:], [eq], [m2], op=ALU.max)
                k.ts(eq.t[:], lj, m2.t[:, j:j + 1], None, ALU.is_ge, None, [lgt, m2], [eq])
                k.ts(comb.t[:, j, :], lj, m1.t[:, j:j + 1], None, ALU.subtract, None, [lgt, m1], [comb])
                k.act(comb.t[:, j, :], comb.t[:, j, :], AF.Exp, [comb], [comb])
                k.tt(comb.t[:, j, :], comb.t[:, j, :], eq.t[:], ALU.mult, [comb, eq], [comb])
                k.red(m2.t[:, j:j + 1], comb.t[:, j, :], [comb], [m2])
                k.P.op("dve", lambda e, j=j: e.reciprocal(m2.t[:, j:j + 1], m2.t[:, j:j + 1]), [m2], [m2])
                k.ts(comb.t[:, j, :], comb.t[:, j, :], m2.t[:, j:j + 1], None, ALU.mult, None, [comb, m2], [comb])
        nblk = (ntok + 511) // 512
        for e in range(nexp):
            wgu = wgu_d.t[e] if moe else wgu_d.t[0]
            wdn = wdn_d.t[e] if moe else wdn_d.t[0]
            wguv = wgu.rearrange("(c p) n -> p c n", p=128)
            for f8 in range(8):
                for c in range(7):
                    fc = (f8 * 7 + c) * 128
                    w = wgb[wi[0] % 2]
                    wi[0] += 1
                    k.dma("pool", w.t[:, :, 0:128], wguv[:, :, fc:fc + 128], [wgu_d], [w])
                    k.dma("pool", w.t[:, :, 128:256], wguv[:, :, DFF + fc:DFF + fc + 128], [wgu_d], [w])
                    for tb in range(nblk):
                        tn = min(512, ntok - tb * 512)
                        hbufs = [h2Tb[jj] for jj in range(tb * 4, min(nt, tb * 4 + 4))]
                        pg, pgf = pbank()
                        for kc in range(16):
                            k.mm(pg[:, 0:tn], w.t[:, kc, 0:128], h2T.t[:, kc, tb * 512:tb * 512 + tn], kc == 0, kc == 15, [w] + hbufs, pgf)
                        pu, puf = pbank()
                        for kc in range(16):
                            k.mm(pu[:, 0:tn], w.t[:, kc, 128:256], h2T.t[:, kc, tb * 512:tb * 512 + tn], kc == 0, kc == 15, [w] + hbufs, puf)
                        sg = sgb[(c * nblk + tb) % 2]
                        k.act(sg.t[:, 0:tn], pg[:, 0:tn], AF.Silu, pgf, [sg])
                        k.tt(actT.t[:, c, tb * 512:tb * 512 + tn], sg.t[:, 0:tn], pu[:, 0:tn], ALU.mult, [sg] + puf, [actT])
                for dg in range(4):
                    wd = wdb[wi[1] % 2]
                    wi[1] += 1
                    k.dma("pool", wd.t[:], wdn[f8 * 896:(f8 + 1) * 896, dg * 512:(dg + 1) * 512].rearrange("(c p) n -> p c n", p=128), [wdn_d], [wd])
                    for j in range(nt):
                        po, pof = k.bank1()
                        for c in range(7):
                            k.mm(po, actT.t[:, c, j * 128:(j + 1) * 128], wd.t[:, c, :], c == 0, c == 6, [actT, wd], pof)
                        t_ = tmpb[j % 2]
                        g2s = G2.t[:, dg * 512:(dg + 1) * 512]
                        if moe:
                            k.stt(t_.t[:], po, comb.t[:, j, e:e + 1], g2s, ALU.mult, ALU.mult, pof + [comb, G2], [t_])
                        else:
                            k.tt(t_.t[:], po, g2s, ALU.mult, pof + [G2], [t_])
                        xs = xacc[j].t[:, dg * 512:(dg + 1) * 512]
                        k.tt(xs, xs, t_.t[:], ALU.add, [xacc[j], t_], [xacc[j]])
        for j in range(nt):
            ti = t0 + j
            k.dma("sp", xout.t[ti * 128:(ti + 1) * 128, :], xacc[j].t[:], [xacc[j]], [xout], store=True)


def pkg_specs(l):
    L = str(l)
    return {
        "mod" + L: ([2, 12288], F32), "xch" + L: ([NX], BF16),
        "cAKT" + L: ([2, 64, CTX], BF16), "cAV" + L: ([CTX, 130], BF16), "cBKT" + L: ([8, 96, CTX], BF16),
        "cBV" + L: ([CTX, 520], BF16), "cCK" + L: ([CTX, 512], BF16), "cCV" + L: ([CTX, 512], BF16),
        "cCKT" + L: ([8, 64, CTX], BF16), "cDKT" + L: ([8, 64, CTX], BF16), "cDV" + L: ([CTX, 520], BF16),
        "AQT" + L: ([8, 64, T], BF16), "BQT" + L: ([8, 96, T], BF16), "CQT" + L: ([8, 64, T], BF16),
        "CKT" + L: ([8, 64, LAT], BF16), "CG" + L: ([T, 512], BF16), "DQT" + L: ([8, 64, T], BF16),
    }


B_CONST_SPECS = {
    "c_ident": ([128, 128], F32), "c_amask": ([4, 128, 128], F32), "c_dmask": ([len(D_COMBOS), 128, 128], F32),
    "c_pos": ([128, 4], F32), "c_flags": ([128, 2], F32), "c_ul": ([2, 128, 128], F32),
}


def b_core_consts(hf):
    m = {"c_ident": np.eye(128, dtype=np.float32), "c_amask": a_masks_np(hf), "c_dmask": d_masks_np(hf)}
    m.update(c_consts_np(hf))
    return m


_DBI = d_bias_index()


def dbias_np(rpb_l):
    flat = rpb_l.reshape(8, 15 * 31)
    g = flat[:, _DBI]
    return np.ascontiguousarray(g.transpose(1, 2, 0, 3)).astype(np.float32)


def _run(nc, k, in_maps):
    names = [n for n in k.dram if n in k.ext_in]
    maps = [{n: m[n] for n in names} for m in in_maps]
    res = run_bass_kernel_spmd(nc, maps, core_ids=list(range(len(maps))))
    return res.results


def kernel(**inputs):
    inputs = {kk: np.asarray(v) for kk, v in inputs.items()}
    ncores = 8
    cores = [(c // 2, c % 2) for c in range(ncores)]
    xin = [np.concatenate([inputs["x"][b, hf * LAT:(hf + 1) * LAT], inputs["ctx"][b]], axis=0).astype(np.float32)
           for (b, hf) in cores]
    out = None
    for l in range(2):
        L = str(l)
        last = l == 1
        lay = layer_in_arrays(inputs, l)
        ext_in = dict(CONST_SPECS)
        ext_in.update(layer_in_specs(l))
        ext_in["cc"] = ([2, D], F32)
        ext_in["xin" + L] = ([T, D], F32)
        outsA = list(pkg_specs(l).keys())
        nc, k = build([lambda k, l=l: phase_A(k, l)], ext_in, outsA)
        maps = []
        for ci, (b, hf) in enumerate(cores):
            m = core_consts(hf)
            m.update(lay)
            m["cc"] = np.stack([inputs["c"][b], inputs["c_ctx"]]).astype(np.float32)
            m["xin" + L] = xin[ci]
            maps.append(m)
        resA = _run(nc, k, maps)
        ext_in = dict(B_CONST_SPECS)
        ext_in.update(layer_in_specs(l))
        ext_in.update(pkg_specs(l))
        ext_in["xin" + L] = ([T, D], F32)
        ext_in["gx" + L] = ([2, NX], BF16)
        ext_in["dbias" + L] = ([7, 128, 8, 128], F32)
        if l % 2 == 0:
            ext_in["ffn_w_gate_up"] = ([1, D, 2 * DFF], F32)
            ext_in["ffn_w_down"] = ([1, DFF, D], F32)
        else:
            ext_in["moe_router"] = ([D, NEXP], F32)
            ext_in["moe_w_gate_up"] = ([NEXP, D, 2 * DFF], F32)
            ext_in["moe_w_down"] = ([NEXP, DFF, D], F32)
        nc, k = build([lambda k, l=l, last=last: phase_B1(k, l, last), lambda k, l=l, last=last: phase_B2(k, l, last),
                       lambda k, l=l, last=last: phase_B3(k, l, last)], ext_in, ["xout" + L])
        dbl_ = dbias_np(inputs["d_rpb"][l])
        maps = []
        for ci, (b, hf) in enumerate(cores):
            m = b_core_consts(hf)
            m.update(lay)
            for n in pkg_specs(l):
                m[n] = resA[ci][n]
            m["xin" + L] = xin[ci]
            m["gx" + L] = np.stack([resA[2 * b][("xch" + L)], resA[2 * b + 1]["xch" + L]])
            m["dbias" + L] = dbl_
            if l % 2 == 0:
                m["ffn_w_gate_up"] = inputs["ffn_w_gate_up"][l // 2:l // 2 + 1]
                m["ffn_w_down"] = inputs["ffn_w_down"][l // 2:l // 2 + 1]
            else:
                m["moe_router"] = inputs["moe_router"][l // 2]
                m["moe_w_gate_up"] = inputs["moe_w_gate_up"][l // 2]
                m["moe_w_down"] = inputs["moe_w_down"][l // 2]
            maps.append(m)
        resB = _run(nc, k, maps)
        xin = [np.asarray(resB[ci]["xout" + L]) for ci in range(ncores)]
    out = np.zeros((4, S_FULL, D), dtype=np.float32)
    for ci, (b, hf) in enumerate(cores):
        out[b, hf * LAT:(hf + 1) * LAT] = xin[ci][:LAT]
    return out
```
